# Optimizing a Trainium2 kernel written in Bass

```python
import jax, jax.numpy as jnp
from jax import lax
import numpy as np

D_MODEL = 1024
BATCH = 16
SEQ = 2048
DEPTH = 1

D_INNER = 2 * D_MODEL
GLA_WIDTH = D_INNER // 2
MLSTM_WIDTH = D_INNER - GLA_WIDTH
GLA_HEADS = 4
GLA_KEY_WIDTH = GLA_WIDTH // 2
GLA_DK = GLA_KEY_WIDTH // GLA_HEADS
GLA_DV = GLA_WIDTH // GLA_HEADS
GLA_GATE_RANK = 16
GLA_GATE_TAU = 16.0
MLSTM_HEADS = 4
MLSTM_DH = MLSTM_WIDTH // MLSTM_HEADS
QKV_BLOCK = 4
QKV_NBLOCKS = MLSTM_WIDTH // QKV_BLOCK
CONV_WIDTH = 4
CHUNK = 64
EPS = 1e-6
SPLIT_SIZES = (GLA_KEY_WIDTH, GLA_KEY_WIDTH, GLA_WIDTH, GLA_WIDTH, GLA_GATE_RANK, MLSTM_WIDTH, MLSTM_WIDTH)
PROJ_WIDTH = sum(SPLIT_SIZES)

kernel_name = "hymba_style_gla_mlstm_hybrid"


def rms_norm(x, g):
    xf = x.astype(jnp.float32)
    y = xf * lax.rsqrt(jnp.mean(xf * xf, axis=-1, keepdims=True) + EPS)
    return (y * g.astype(jnp.float32)).astype(x.dtype)


def to_chunks(t):
    b, s = t.shape[0], t.shape[1]
    t = t.reshape((b, s // CHUNK, CHUNK) + t.shape[2:])
    if t.ndim == 5:
        return t.transpose(1, 0, 3, 2, 4)
    return t.transpose(1, 0, 3, 2)


def from_chunks(t):
    n, b, h, c, d = t.shape
    return t.transpose(1, 0, 3, 2, 4).reshape(b, n * c, h, d)


def gla_chunked(q, k, v, log_a):
    bsz = q.shape[0]
    qc = to_chunks(q.astype(jnp.float32) * (GLA_DK ** -0.5))
    kc = to_chunks(k.astype(jnp.float32))
    vc = to_chunks(v.astype(jnp.float32))
    bc = jnp.cumsum(to_chunks(log_a.astype(jnp.float32)), axis=3)
    causal = jnp.tril(jnp.ones((CHUNK, CHUNK), dtype=bool))

    def step(state, inp):
        q_, k_, v_, b_ = inp
        diff = b_[:, :, :, None, :] - b_[:, :, None, :, :]
        decay = jnp.exp(jnp.where(causal[:, :, None], diff, -jnp.inf))
        scores = jnp.einsum('bhid,bhjd,bhijd->bhij', q_, k_, decay)
        o = jnp.einsum('bhij,bhje->bhie', scores, v_) + \
            jnp.einsum('bhid,bhde->bhie', q_ * jnp.exp(b_), state)
        b_last = b_[:, :, -1:, :]
        k_dec = k_ * jnp.exp(b_last - b_)
        state = jnp.exp(b_last[:, :, 0, :])[..., None] * state + \
            jnp.einsum('bhjd,bhje->bhde', k_dec, v_)
        return state, o

    state0 = jnp.zeros((bsz, GLA_HEADS, GLA_DK, GLA_DV), jnp.float32)
    _, o = lax.scan(step, state0, (qc, kc, vc, bc))
    return from_chunks(o)


def mlstm_chunked(q, k, v, i_pre, f_pre):
    bsz = q.shape[0]
    qc = to_chunks(q.astype(jnp.float32))
    kc = to_chunks(k.astype(jnp.float32) * (MLSTM_DH ** -0.5))
    vc = to_chunks(v.astype(jnp.float32))
    ic = to_chunks(i_pre.astype(jnp.float32))
    bc = jnp.cumsum(to_chunks(jax.nn.log_sigmoid(f_pre.astype(jnp.float32))), axis=-1)
    causal = jnp.tril(jnp.ones((CHUNK, CHUNK), dtype=bool))

    def step(carry, inp):
        c_hat, n_hat, m_prev = carry
        q_, k_, v_, b_, i_ = inp
        log_d = b_[..., :, None] - b_[..., None, :] + i_[..., None, :]
        log_d = jnp.where(causal, log_d, -jnp.inf)
        log_inter = b_ + m_prev[..., None]
        m = jnp.maximum(log_inter, jnp.max(log_d, axis=-1))
        s = jnp.einsum('bhid,bhjd->bhij', q_, k_) * jnp.exp(log_d - m[..., None])
        w_inter = jnp.exp(log_inter - m)
        num = jnp.einsum('bhij,bhje->bhie', s, v_) + \
            w_inter[..., None] * jnp.einsum('bhid,bhed->bhie', q_, c_hat)
        den = jnp.sum(s, axis=-1) + w_inter * jnp.einsum('bhid,bhd->bhi', q_, n_hat)
        h = num / jnp.maximum(jnp.abs(den), jnp.exp(-m))[..., None]
        m_new = m[..., -1]
        decay_prev = jnp.exp(b_[..., -1] + m_prev - m_new)
        wk = jnp.exp(b_[..., -1:] - b_ + i_ - m_new[..., None])
        c_new = decay_prev[..., None, None] * c_hat + jnp.einsum('bhj,bhje,bhjd->bhed', wk, v_, k_)
        n_new = decay_prev[..., None] * n_hat + jnp.einsum('bhj,bhjd->bhd', wk, k_)
        return (c_new, n_new, m_new), h

    carry0 = (jnp.zeros((bsz, MLSTM_HEADS, MLSTM_DH, MLSTM_DH), jnp.float32),
              jnp.zeros((bsz, MLSTM_HEADS, MLSTM_DH), jnp.float32),
              jnp.zeros((bsz, MLSTM_HEADS), jnp.float32))
    _, h = lax.scan(step, carry0, (qc, kc, vc, bc, ic))
    return from_chunks(h)


def headwise(t, w):
    b, s, _ = t.shape
    tb = t.reshape(b, s, QKV_NBLOCKS, QKV_BLOCK)
    return jnp.einsum('bsnd,nde->bsne', tb, w).reshape(b, s, MLSTM_WIDTH)


def head_rms_norm(t, g):
    return rms_norm(t, g)


def setup_inputs(seed: int = 0) -> dict:
    key = jax.random.key(seed)
    ks = jax.random.split(key, 24)
    f32 = jnp.float32
    nrm = lambda k, shape, scale: jax.random.normal(k, shape, f32) * scale
    gain = lambda k, shape: 1.0 + 0.02 * jax.random.normal(k, shape, f32)
    return {
        "x": jax.random.normal(ks[0], (BATCH, SEQ, D_MODEL), f32),
        "norm_g": gain(ks[1], (D_MODEL,)),
        "w_in": nrm(ks[2], (D_MODEL, PROJ_WIDTH), D_MODEL ** -0.5),
        "w_gla_gate_up": nrm(ks[3], (GLA_GATE_RANK, GLA_KEY_WIDTH), GLA_GATE_RANK ** -0.5),
        "b_gla_gate": nrm(ks[4], (GLA_KEY_WIDTH,), 0.1),
        "gla_norm_g": gain(ks[5], (GLA_HEADS, GLA_DV)),
        "conv_w": nrm(ks[6], (CONV_WIDTH, MLSTM_WIDTH), CONV_WIDTH ** -0.5),
        "conv_b": nrm(ks[7], (MLSTM_WIDTH,), 0.02),
        "w_q_m": nrm(ks[8], (QKV_NBLOCKS, QKV_BLOCK, QKV_BLOCK), QKV_BLOCK ** -0.5),
        "w_k_m": nrm(ks[9], (QKV_NBLOCKS, QKV_BLOCK, QKV_BLOCK), QKV_BLOCK ** -0.5),
        "w_v_m": nrm(ks[10], (QKV_NBLOCKS, QKV_BLOCK, QKV_BLOCK), QKV_BLOCK ** -0.5),
        "w_igate": nrm(ks[11], (3 * MLSTM_WIDTH, MLSTM_HEADS), (3 * MLSTM_WIDTH) ** -0.5),
        "b_igate": nrm(ks[12], (MLSTM_HEADS,), 0.1),
        "w_fgate": nrm(ks[13], (3 * MLSTM_WIDTH, MLSTM_HEADS), (3 * MLSTM_WIDTH) ** -0.5),
        "b_fgate": jnp.linspace(3.0, 6.0, MLSTM_HEADS, dtype=f32) + nrm(ks[14], (MLSTM_HEADS,), 0.01),
        "mlstm_norm_g": gain(ks[15], (MLSTM_HEADS, MLSTM_DH)),
        "mlstm_skip": gain(ks[16], (MLSTM_WIDTH,)),
        "w_out": nrm(ks[17], (D_INNER, D_MODEL), D_INNER ** -0.5),
        "final_norm_g": gain(ks[18], (D_MODEL,)),
    }


def reference(x, norm_g, w_in, w_gla_gate_up, b_gla_gate, gla_norm_g, conv_w, conv_b,
              w_q_m, w_k_m, w_v_m, w_igate, b_igate, w_fgate, b_fgate,
              mlstm_norm_g, mlstm_skip, w_out, final_norm_g):
    bsz, seq, _ = x.shape
    for _layer in range(DEPTH):
        u = rms_norm(x, norm_g)
        proj = u @ w_in
        cuts = list(np.cumsum(SPLIT_SIZES)[:-1])
        q_g, k_g, v_g, z_g, r_g, x_m, z_m = jnp.split(proj, cuts, axis=-1)

        log_a = jax.nn.log_sigmoid((r_g @ w_gla_gate_up + b_gla_gate).astype(jnp.float32)) / GLA_GATE_TAU
        o_g = gla_chunked(q_g.reshape(bsz, seq, GLA_HEADS, GLA_DK),
                          k_g.reshape(bsz, seq, GLA_HEADS, GLA_DK),
                          v_g.reshape(bsz, seq, GLA_HEADS, GLA_DV),
                          log_a.reshape(bsz, seq, GLA_HEADS, GLA_DK))
        o_g = head_rms_norm(o_g, gla_norm_g).reshape(bsz, seq, GLA_WIDTH).astype(x.dtype)
        o_g = o_g * jax.nn.silu(z_g)

        conv = lax.conv_general_dilated(
            x_m, conv_w[:, None, :].astype(x_m.dtype), window_strides=(1,),
            padding=[(CONV_WIDTH - 1, 0)], dimension_numbers=('NWC', 'WIO', 'NWC'),
            feature_group_count=MLSTM_WIDTH)
        c_act = jax.nn.silu(conv + conv_b)
        q_m = headwise(c_act, w_q_m)
        k_m = headwise(c_act, w_k_m)
        v_m = headwise(x_m, w_v_m)
        qkv = jnp.concatenate([q_m, k_m, v_m], axis=-1)
        i_pre = qkv @ w_igate + b_igate
        f_pre = qkv @ w_fgate + b_fgate
        h_m = mlstm_chunked(q_m.reshape(bsz, seq, MLSTM_HEADS, MLSTM_DH),
                            k_m.reshape(bsz, seq, MLSTM_HEADS, MLSTM_DH),
                            v_m.reshape(bsz, seq, MLSTM_HEADS, MLSTM_DH),
                            i_pre, f_pre)
        h_m = head_rms_norm(h_m, mlstm_norm_g).reshape(bsz, seq, MLSTM_WIDTH).astype(x.dtype)
        o_m = (h_m + mlstm_skip * c_act) * jax.nn.silu(z_m)

        x = x + jnp.concatenate([o_g, o_m], axis=-1) @ w_out
    return rms_norm(x, final_norm_g)
```

```python
import contextlib
import math
import numpy as np
import concourse.bass as bass
import concourse.mybir as mybir
from concourse.bass_utils import run_bass_kernel_spmd

F32 = mybir.dt.float32
BF16 = mybir.dt.bfloat16
AF = mybir.ActivationFunctionType
ALU = mybir.AluOpType

D = 1024
PW = 5136
NCORES = 8
ENGS = ("pe", "act", "dve", "pool", "sp")
EPS = 1e-6
LN16 = math.log(16.0)


class Prog:
    def __init__(self, nc):
        self.nc = nc
        self.ops = []
        self.last_use = {}

    def op(self, eng, fn, reads=(), writes=(), chan=None):
        for k in tuple(reads) + tuple(writes):
            if k.startswith("ps"):
                self.last_use[k] = len(self.ops)
        self.ops.append((eng, fn, tuple(reads), tuple(writes), chan))

    def emit(self):
        nc, ops = self.nc, self.ops
        n = len(ops)
        deps = [set() for _ in range(n)]
        last_w, readers = {}, {}
        needs_inc = [False] * n
        for i, (eng, fn, reads, writes, chan) in enumerate(ops):
            for k in reads:
                if k in last_w:
                    deps[i].add(last_w[k])
            for k in writes:
                if k in last_w:
                    deps[i].add(last_w[k])
                for r in readers.get(k, ()):
                    if r != i:
                        deps[i].add(r)
            for k in reads:
                readers.setdefault(k, []).append(i)
            for k in writes:
                last_w[k] = i
                readers[k] = []

        def semkey(i):
            eng, _, _, _, chan = ops[i]
            return ("D", chan) if chan is not None else ("E", eng)

        for i in range(n):
            eng = ops[i][0]
            rm = set()
            for j in deps[i]:
                if ops[j][4] is None and ops[i][4] is None and ops[j][0] == "pe" and eng == "pe":
                    rm.add(j)
            deps[i] -= rm
            last = {}
            for j in deps[i]:
                sk = semkey(j)
                if sk not in last or j > last[sk]:
                    last[sk] = j
            deps[i] = set(last.values())
            for j in deps[i]:
                needs_inc[j] = True
        for i in range(n):
            if ops[i][4] is not None:
                needs_inc[i] = True
        counts, ticket = {}, [None] * n
        for i in range(n):
            if needs_inc[i]:
                sk = semkey(i)
                counts[sk] = counts.get(sk, 0) + (16 if sk[0] == "D" else 1)
                ticket[i] = (sk, counts[sk])
        with contextlib.ExitStack() as st:
            sems = {sk: st.enter_context(nc.semaphore("s_%s_%s" % sk)) for sk in sorted(counts)}
            block = st.enter_context(nc.Block())
            handles = {"pe": block.tensor, "act": block.scalar, "dve": block.vector,
                       "pool": block.gpsimd, "sp": block.sync}
            for eng in ENGS:
                my = [i for i in range(n) if ops[i][0] == eng]
                if not my:
                    continue

                def body(e, my=my):
                    waited = {}
                    for i in my:
                        for j in sorted(deps[i]):
                            sk, v = ticket[j]
                            if waited.get(sk, 0) < v:
                                e.wait_ge(sems[sk], v)
                                waited[sk] = v
                        ins = ops[i][1](e)
                        if needs_inc[i]:
                            sk, v = ticket[i]
                            ins.then_inc(sems[sk], 16 if sk[0] == "D" else 1)

                handles[eng](body)


def build(nseq=2, ntile=16):
    ntok = nseq * ntile * 128
    nc = bass.Bass("TRN2", target_bir_lowering=False)
    dram = lambda name, shape, kind="ExternalInput": nc.dram_tensor(name, shape, F32, kind=kind).ap()
    x_d = dram("x", [ntok, D])
    win_d = dram("w_in", [D, PW])
    wout_d = dram("w_out", [2 * D, D])
    p128_d = dram("p128", [128, 64])
    rows_d = dram("rows", [1, 1032])
    wup_d = dram("wup", [128, 512])
    wq_d = dram("wq", [D, 128])
    wk_d = dram("wk", [D, 128])
    wv_d = dram("wv", [D, 128])
    wg_d = dram("wg", [128, 192])
    gmn_d = dram("gmn", [1, D])
    gfin_d = dram("gfin", [1, D])
    out_d = dram("out", [ntok, D], kind="ExternalOutput")

    st = contextlib.ExitStack()
    with st:
        sb = lambda name, shape, dt: st.enter_context(nc.sbuf_tensor(name, shape, dt))
        P = Prog(nc)
        win = sb("win", [128, 8, PW], BF16)
        wout = sb("wout", [128, 16, D], BF16)
        Wq = sb("Wq", [128, 8, 128], BF16)
        Wk = sb("Wk", [128, 8, 128], BF16)
        Wv = sb("Wv", [128, 8, 128], BF16)
        dconv = sb("dconv", [128, 4, 8, 128], BF16)
        dskip = sb("dskip", [128, 8, 128], BF16)
        wg = sb("wgt", [128, 24, 8], BF16)
        gmn = sb("gmn_bc", [128, D], F32)
        gfin = sb("gfin_bc", [128, D], F32)
        p128 = sb("p128t", [128, 64], F32)
        rowsb = sb("rowsb", [1, 1032], BF16)
        wup = sb("wupt", [128, 512], BF16)
        identf = sb("identf", [128, 128], F32)
        ident = sb("ident", [128, 128], BF16)
        maskf = sb("maskf", [128, 128], F32)
        maskb = sb("maskb", [128, 128], BF16)
        triG = sb("triG", [128, 128], BF16)
        UG = sb("UG", [128, 128], BF16)
        triN = sb("triN", [128, 128], F32)
        UN = sb("UN", [128, 128], F32)
        onesN = sb("onesN", [128, 128], F32)
        ones_row = sb("ones_row", [1, 128], BF16)
        raug = sb("raug", [128, 128], BF16)
        gbb = sb("gbb", [128, 8], BF16)
        Sg32 = sb("Sg32", [128, 4, 256], F32)
        Sgb = sb("Sgb", [128, 4, 256], BF16)
        Cm32 = sb("Cm32", [128, 8, 257], F32)
        Cmb = sb("Cmb", [128, 8, 257], BF16)
        xt = sb("xt", [128, D], F32)
        xs = sb("xs", [128, D], BF16)
        xT = sb("xT", [128, 8, 128], BF16)
        xme = sb("xme", [128, 8, 131], BF16)
        vg = sb("vg", [128, D], BF16)
        szg = sb("szg", [128, D], BF16)
        szm = sb("szm", [128, D], BF16)
        scrA = sb("scrA", [128, 1024], F32)
        scrB = sb("scrB", [128, 512], F32)
        qs = sb("qs", [128, 4, 128], BF16)
        ks = sb("ks", [128, 4, 128], BF16)
        kdec = sb("kdec", [128, 512], BF16)
        spb = kdec
        cactT = sb("cactT", [128, 8, 128], BF16)
        qmT = sb("qmT", [128, 8, 128], BF16)
        kmT = sb("kmT", [128, 8, 128], BF16)
        smb = sb("smb", [128, 8, 128], BF16)
        sm = smb[:, 0:4, :]
        smm = smb[:, 4:8, :]
        vaug = sb("vaug", [128, 4, 257], BF16)
        kw = sb("kw", [128, D], BF16)
        obf = sb("obf", [128, 2 * D], BF16)
        oT = sb("oT", [128, 16, 128], BF16)
        yt = obf[:, :].bitcast(F32)
        junk = oT[:, 0:8, :].rearrange("p c t -> p (c t)")
        vmT = oT[:, 0:8, :]
        junkh = [oT[:, 8 + 2 * h:10 + 2 * h, :].rearrange("p c t -> p (c t)") for h in range(4)]
        OTK = ["oT0", "oT1_0", "oT1_1", "oT1_2", "oT1_3"]
        st_x = sb("st_x", [128, 4], F32)
        st_g = sb("st_g", [128, 12], F32)
        gl = sb("gl", [128, 8], F32)
        gtmp = sb("gtmp", [128, 4], F32)
        gex = sb("gex", [128, 16], F32)
        st_m = sb("st_m", [128, 40], F32)
        st_y = sb("st_y", [128, 4], F32)
        dmy = sb("dmy", [128, 4], F32)

        psf, psb = [], []
        for b in range(8):
            t = st.enter_context(nc.psum_tensor("ps%d" % b, [128, 512], F32))
            psf.append(t)
            psb.append(t.bitcast(BF16))
        psf += [psf[7][:, 0:128], psf[7][:, 128:144], psf[7][:, 144:160], psf[7][:, 160:168]]
        pctr = [0]
        PK = lambda b: "ps%d" % min(b, 7)

        held = set()

        def palloc(avoid=(), hold=False):
            cands = [b for b in range(7) if b not in avoid and b not in held]
            assert cands, "out of PSUM banks"
            b = min(cands, key=lambda b: P.last_use.get("ps%d" % b, -1 - (8 - b)))
            P.last_use["ps%d" % b] = len(P.ops)
            if hold:
                held.add(b)
            return b

        def prel(*banks):
            for b in banks:
                held.discard(b)

        P.op("pool", lambda e: e.memset(identf[:], 0.0), writes=["identf"])
        P.op("pool", lambda e: e.affine_select(out=identf[:], in_=identf[:], pattern=[[-1, 128]],
                                               compare_op=ALU.not_equal, fill=1.0, base=0, channel_multiplier=1),
             reads=["identf"], writes=["identf"])
        P.op("pool", lambda e: e.memset(maskf[:], 1.0), writes=["maskf"])
        P.op("pool", lambda e: e.affine_select(out=maskf[:], in_=maskf[:], pattern=[[1, 128]],
                                               compare_op=ALU.is_ge, fill=0.0, base=0, channel_multiplier=-1),
             reads=["maskf"], writes=["maskf"])
        WGRP = [(3072, 4112), (2048, 3072), (4112, 5136), (0, 1024), (1024, 2048)]

        def load_wgroup(gi):
            c0, c1 = WGRP[gi]
            for k in range(8):
                P.op("pool", lambda e, k=k, c0=c0, c1=c1: e.dma_start(out=win[:, k, c0:c1], in_=win_d[k * 128:(k + 1) * 128, c0:c1],
                                                                      max_dma_last_dim=4096),
                     writes=["win%d_%d" % (gi, k)], chan="win%d_%d" % (gi, k))

        def wkey(col0, k):
            for gi, (c0, c1) in enumerate(WGRP):
                if c0 <= col0 < c1:
                    return "win%d_%d" % (gi, k)
            raise KeyError(col0)
        P.op("sp", lambda e: e.dma_start(out=p128[:], in_=p128_d), writes=["p128"], chan="p128")
        P.op("pool", lambda e: e.dma_start(out=wup[:], in_=wup_d), writes=["wup"], chan="wup")
        load_wgroup(0)
        P.op("pool", lambda e: e.dma_start(out=rowsb[:], in_=rows_d), writes=["rowsb"], chan="rows")
        P.op("sp", lambda e: e.dma_start(out=gmn[:], in_=gmn_d.partition_broadcast(128)), writes=["gmn"], chan="gmn")
        P.op("sp", lambda e: e.dma_start(out=gfin[:], in_=gfin_d.partition_broadcast(128)), writes=["gfin"], chan="gfin")
        load_wgroup(1)
        load_wgroup(2)
        load_wgroup(3)
        P.op("pool", lambda e: e.dma_start(out=Wq[:], in_=wq_d.rearrange("(c p) n -> p c n", p=128)), writes=["Wq"], chan="Wq")
        P.op("pool", lambda e: e.dma_start(out=Wk[:], in_=wk_d.rearrange("(c p) n -> p c n", p=128)), writes=["Wk"], chan="Wk")
        P.op("pool", lambda e: e.dma_start(out=Wv[:], in_=wv_d.rearrange("(c p) n -> p c n", p=128)), writes=["Wv"], chan="Wv")
        P.op("pool", lambda e: e.dma_start(out=wg[:], in_=wg_d.rearrange("p (c j) -> p c j", j=8)), writes=["wg"], chan="wg")
        load_wgroup(4)
        for kk in range(2):
            P.op("pool", lambda e, kk=kk: e.dma_start(
                out=wout[:, kk * 8:(kk + 1) * 8, :],
                in_=wout_d[kk * 1024:(kk + 1) * 1024, :].rearrange("(c p) n -> p c n", p=128)),
                writes=["wout%d" % kk], chan="wout%d" % kk)

        P.op("dve", lambda e: e.tensor_copy(out=ident[:], in_=identf[:]), reads=["identf"], writes=["ident"])
        P.op("dve", lambda e: e.tensor_copy(out=maskb[:], in_=maskf[:]), reads=["maskf"], writes=["maskb"])
        P.op("dve", lambda e: e.tensor_scalar(out=triG[:], in0=maskf[:], scalar1=-1.0 / 16, scalar2=None, op0=ALU.mult),
             reads=["maskf"], writes=["triG"])
        P.op("dve", lambda e: e.tensor_scalar(out=UG[:], in0=maskf[:], scalar1=-1.0, scalar2=1.0 / 16, op0=ALU.add, op1=ALU.mult),
             reads=["maskf"], writes=["UG"])
        P.op("dve", lambda e: e.tensor_scalar(out=triN[:], in0=maskf[:], scalar1=-1.0, scalar2=None, op0=ALU.mult),
             reads=["maskf"], writes=["triN"])
        P.op("dve", lambda e: e.tensor_scalar(out=UN[:], in0=maskf[:], scalar1=-1.0, scalar2=None, op0=ALU.add),
             reads=["maskf"], writes=["UN"])
        P.op("dve", lambda e: e.memset(onesN[:], -1.0), writes=["onesN"])
        P.op("dve", lambda e: e.memset(dmy[:], 0.0), writes=["dmy0", "dmy1", "dmy2", "dmy3"])
        P.op("dve", lambda e: e.memset(ones_row[:], 1.0), writes=["ones_row"])
        P.op("dve", lambda e: e.memset(raug[:], 1.0), writes=["raug"])
        P.op("dve", lambda e: e.memset(vaug[:], 1.0), writes=["vaug"])
        def init_diag(ks, with_skip):
            for k in ks:
                for c in range(8):
                    P.op("dve", lambda e, k=k, c=c: e.tensor_scalar(out=dconv[:, k, c, :], in0=identf[:],
                                                                     scalar1=p128[:, 24 + k * 8 + c:25 + k * 8 + c],
                                                                     scalar2=None, op0=ALU.mult),
                         reads=["identf", "p128"], writes=["dconv"])
            if with_skip:
                for c in range(8):
                    P.op("dve", lambda e, c=c: e.tensor_scalar(out=dskip[:, c, :], in0=identf[:], scalar1=p128[:, 16 + c:17 + c],
                                                                scalar2=None, op0=ALU.mult),
                         reads=["identf", "p128"], writes=["dskip"])

        init_diag([0, 1], False)

        gT = p128[:, 0:8]
        convb_row = rowsb[0:1, 0:1024]
        gbias_row = rowsb[0:1, 1024:1032]

        def load_x(g):
            P.op("sp", lambda e, g=g: e.dma_start(out=xt[:], in_=x_d[g * 128:(g + 1) * 128, :]),
                 writes=["xt"], chan="xt")

        load_x(0)
        out_keys = []
        def tile(g, s, ti):
            if True:
                first = (ti == 0)
                P.op("act", lambda e: e.activation(out=xs[:], in_=xt[:], func=AF.Square, accum_out=st_x[:, 0:1]),
                     reads=["xt"], writes=["st_x0", "xs"])
                P.op("act", lambda e: e.activation(out=st_x[:, 1:2], in_=st_x[:, 0:1], func=AF.Ln, scale=1.0 / D, bias=EPS),
                     reads=["st_x0"], writes=["st_x1"])
                P.op("act", lambda e: e.activation(out=st_x[:, 2:3], in_=st_x[:, 1:2], func=AF.Exp, scale=-0.5),
                     reads=["st_x1"], writes=["st_x2"])
                P.op("dve", lambda e: e.tensor_scalar(out=xs[:], in0=xt[:], scalar1=st_x[:, 2:3], scalar2=None, op0=ALU.mult),
                     reads=["xt", "st_x2"], writes=["xs"])
                if g + 1 < nseq * ntile:
                    load_x(g + 1)
                yield "A1"
                bT_ = palloc()
                for c in range(8):
                    P.op("pe", lambda e, c=c, b=bT_: e.transpose(out=psb[b][:, c * 128:(c + 1) * 128],
                                                                  in_=xs[:, c * 128:(c + 1) * 128], identity=ident[:]),
                         reads=["xs", "ident"], writes=["ps%d" % bT_])
                P.op("dve", lambda e, b=bT_: e.tensor_tensor(
                    out=xT[:], in0=psb[b][:, 0:1024].rearrange("p (c t) -> p c t", c=8),
                    in1=gT.unsqueeze(2).to_broadcast([128, 8, 128]), op=ALU.mult),
                    reads=["ps%d" % bT_, "p128"], writes=["xT"])

                yield "A"
                def proj_fm(col0, nch, M=128, bank=None, hold=False):
                    b = palloc(hold=hold) if bank is None else bank
                    for ch in range(nch):
                        for k in range(8):
                            P.op("pe", lambda e, ch=ch, k=k, b=b: e.matmul(
                                psf[b][0:M, ch * 128:(ch + 1) * 128],
                                lhsT=win[:, k, col0 + ch * M: col0 + (ch + 1) * M], rhs=xT[:, k, :],
                                start=(k == 0), stop=(k == 7)),
                                reads=["xT", wkey(col0, k)], writes=[PK(b)])
                    return b

                b_r = proj_fm(3072, 1, M=128, bank=8)
                P.op("dve", lambda e, b=b_r: e.tensor_copy(out=raug[0:16, :], in_=psf[b][0:16, 0:128]),
                     reads=[PK(b_r)], writes=["raug"])
                if first:
                    P.op("pool", lambda e: e.memset(xme[:, :, 0:3], 0.0), writes=["xme_h"])
                else:
                    P.op("pool", lambda e: e.tensor_copy(out=xme[:, :, 0:3], in_=xme[:, :, 128:131]),
                         reads=["xme0", "xme1"], writes=["xme_h"])
                b_x0 = proj_fm(3088, 4)
                b_x1 = proj_fm(3088 + 512, 4)
                P.op("act", lambda e, b=b_x0: e.activation(out=xme[:, 0:4, 3:131], in_=psf[b][:, :].rearrange("p (c t) -> p c t", c=4),
                                                           func=AF.Copy),
                     reads=["ps%d" % b_x0, "xme_h"], writes=["xme0"])
                P.op("act", lambda e, b=b_x1: e.activation(out=xme[:, 4:8, 3:131], in_=psf[b][:, :].rearrange("p (c t) -> p c t", c=4),
                                                           func=AF.Copy),
                     reads=["ps%d" % b_x1, "xme_h"], writes=["xme1"])
                b_pre = palloc()
                P.op("pe", lambda e, b=b_pre: e.matmul(psf[b][:, :], lhsT=raug[:], rhs=wup[:], start=True, stop=True),
                     reads=["raug", "wup"], writes=["ps%d" % b_pre])
                P.op("act", lambda e, b=b_pre: e.activation(out=scrB[:], in_=psf[b][:, :], func=AF.Exp, scale=-1.0),
                     reads=["ps%d" % b_pre], writes=["scrB"])
                P.op("act", lambda e: e.activation(out=spb[:], in_=scrB[:], func=AF.Ln, bias=1.0),
                     reads=["scrB"], writes=["kdec"])
                yield "B1"
                b_cv = []
                for half in range(2):
                    b = palloc()
                    b_cv.append(b)
                    for cc in range(4):
                        c = half * 4 + cc
                        for k in range(4):
                            P.op("pe", lambda e, c=c, cc=cc, k=k, b=b: e.matmul(
                                psf[b][:, cc * 128:(cc + 1) * 128], lhsT=dconv[:, k, c, :], rhs=xme[:, c, k:k + 128],
                                start=(k == 0), stop=(k == 3)),
                                reads=["xme%d" % (c // 4), "xme_h", "dconv"], writes=["ps%d" % b])
                for c in range(8):
                    P.op("act", lambda e, c=c, b=b_cv[c // 4]: e.activation(
                        out=cactT[:, c, :], in_=psf[b][:, (c % 4) * 128:(c % 4 + 1) * 128], func=AF.Silu, bias=p128[:, 56 + c:57 + c]),
                        reads=["ps%d" % b_cv[c // 4], "p128"], writes=["cactT%d" % c])
                yield "CV"
                def proj_tm(col0):
                    b = palloc()
                    for k in range(8):
                        P.op("pe", lambda e, k=k, b=b: e.matmul(psf[b][:, :], lhsT=xT[:, k, :], rhs=win[:, k, col0:col0 + 512],
                                                                start=(k == 0), stop=(k == 7)),
                             reads=["xT", wkey(col0, k)], writes=["ps%d" % b])
                    return b

                for half in range(2):
                    b = proj_tm(2048 + half * 512)
                    P.op("act", lambda e, b=b, half=half: e.activation(out=szg[:, half * 512:(half + 1) * 512], in_=psf[b][:, :], func=AF.Silu),
                         reads=["ps%d" % b], writes=["szg%d" % half])
                for half in range(2):
                    b = proj_tm(4112 + half * 512)
                    P.op("act", lambda e, b=b, half=half: e.activation(out=szm[:, half * 512:(half + 1) * 512], in_=psf[b][:, :], func=AF.Silu),
                         reads=["ps%d" % b], writes=["szm%d" % half])
                yield "Z"
                b_q = proj_fm(0, 4, hold=True)
                b_k = proj_fm(512, 4, hold=True)
                yield "QP"
                b_bT = palloc()
                for h in range(4):
                    P.op("pe", lambda e, h=h, b=b_bT: e.matmul(psf[b][:, h * 128:(h + 1) * 128], lhsT=spb[:, h * 128:(h + 1) * 128],
                                                               rhs=triG[:], start=True, stop=True),
                         reads=["kdec", "triG"], writes=["ps%d" % b_bT])
                b_bL = palloc()
                P.op("pe", lambda e, b=b_bL: e.matmul(psf[b][:, :], lhsT=UG[:], rhs=spb[:], start=True, stop=True),
                     reads=["kdec", "UG"], writes=["ps%d" % b_bL])
                Eq = scrA[:, 0:512]
                Ek = scrA[:, 512:1024]
                P.op("act", lambda e, b=b_bT: e.activation(out=Eq, in_=psf[b][:, :], func=AF.Exp), reads=["ps%d" % b_bT], writes=["A0", "A1"])
                P.op("act", lambda e, b=b_bT: e.activation(out=Ek, in_=psf[b][:, :], func=AF.Exp, scale=-1.0),
                     reads=["ps%d" % b_bT], writes=["A2", "A3"])
                P.op("act", lambda e, b=b_bL: e.activation(out=scrB[:], in_=psf[b][:, :], func=AF.Exp),
                     reads=["ps%d" % b_bL], writes=["scrB"])
                P.op("dve", lambda e, b=b_q: e.scalar_tensor_tensor(out=qs[:].rearrange("p h t -> p (h t)"), in0=psf[b][:, :],
                                                                   scalar=128.0 ** -0.5, in1=Eq, op0=ALU.mult, op1=ALU.mult),
                     reads=["ps%d" % b_q, "A0", "A1"], writes=["qs"])
                P.op("dve", lambda e, b=b_k: e.tensor_tensor(out=ks[:].rearrange("p h t -> p (h t)"), in0=psf[b][:, :], in1=Ek, op=ALU.mult),
                     reads=["ps%d" % b_k, "A2", "A3"], writes=["ks"])
                prel(b_q, b_k)
                yield "C4"
                b_kt = proj_tm(512)
                P.op("dve", lambda e, b=b_kt: e.tensor_tensor(out=kdec[:], in0=psf[b][:, :], in1=scrB[:], op=ALU.mult),
                     reads=["ps%d" % b_kt, "scrB"], writes=["kdec"])
                for half in range(2):
                    b = proj_tm(1024 + half * 512)
                    if half == 0:
                        P.op("act", lambda e, b=b, half=half: e.activation(out=vg[:, half * 512:(half + 1) * 512], in_=psf[b][:, :], func=AF.Copy),
                             reads=["ps%d" % b], writes=["vg%d" % half])
                    else:
                        P.op("dve", lambda e, b=b, half=half: e.tensor_copy(out=vg[:, half * 512:(half + 1) * 512], in_=psf[b][:, :]),
                             reads=["ps%d" % b], writes=["vg%d" % half])
                yield "TM"
                def headwise_fm(Wt, wkey, src, skeys, dst, dkeys, eng="act"):
                    for half in range(2):
                        b = palloc()
                        for cc in range(4):
                            c = half * 4 + cc
                            P.op("pe", lambda e, c=c, cc=cc, b=b: e.matmul(psf[b][:, cc * 128:(cc + 1) * 128], lhsT=Wt[:, c, :],
                                                                           rhs=src(c), start=True, stop=True),
                                 reads=[wkey] + skeys, writes=["ps%d" % b])
                        if eng == "act":
                            P.op("act", lambda e, half=half, b=b: e.activation(
                                out=dst[:, half * 4:(half + 1) * 4, :], in_=psf[b][:, :].rearrange("p (c t) -> p c t", c=4), func=AF.Copy),
                                reads=["ps%d" % b], writes=dkeys(half))
                        else:
                            P.op("dve", lambda e, half=half, b=b: e.tensor_copy(
                                out=dst[:, half * 4:(half + 1) * 4, :], in_=psf[b][:, :].rearrange("p (c t) -> p c t", c=4)),
                                reads=["ps%d" % b], writes=dkeys(half))

                CK = ["cactT%d" % c for c in range(8)]
                headwise_fm(Wq, "Wq", lambda c: cactT[:, c, :], CK, qmT, lambda half: ["qmT%d" % half])
                headwise_fm(Wk, "Wk", lambda c: cactT[:, c, :], CK, kmT, lambda half: ["kmT%d" % half], eng="dve")
                RK = ["R0", "R1", "R2", "R3"]
                headwise_fm(Wv, "Wv", lambda c: xme[:, c, 3:131], ["xme0", "xme1"], vmT, lambda half: ["oT0a"] if half == 0 else ["oT0b"])
                for half in range(2):
                    b = palloc()
                    for cc in range(4):
                        c = half * 4 + cc
                        P.op("pe", lambda e, c=c, cc=cc, b=b: e.matmul(psf[b][:, cc * 128:(cc + 1) * 128], lhsT=xme[:, c, 3:131],
                                                                       rhs=Wv[:, c, :], start=True, stop=True),
                             reads=["xme%d" % (c // 4), "Wv"], writes=["ps%d" % b])
                    if half == 0:
                        P.op("act", lambda e, half=half, b=b: e.activation(
                            out=vaug[:, half * 2:(half + 1) * 2, 0:256], in_=psf[b][:, :].rearrange("p (h e) -> p h e", h=2), func=AF.Copy),
                            reads=["ps%d" % b], writes=["vaug%d" % half])
                    else:
                        P.op("dve", lambda e, half=half, b=b: e.tensor_copy(
                            out=vaug[:, half * 2:(half + 1) * 2, 0:256], in_=psf[b][:, :].rearrange("p (h e) -> p h e", h=2)),
                            reads=["ps%d" % b], writes=["vaug%d" % half])
                if first:
                    P.op("pool", lambda e: e.memset(Sg32[:], 0.0), writes=["Sg32_%d" % h for h in range(4)])
                    P.op("pool", lambda e: e.memset(Sgb[:], 0.0), writes=["Sgb"])
                    P.op("pool", lambda e: e.memset(Cm32[:], 0.0), writes=["Cm32_%d" % c for c in range(8)])
                    P.op("pool", lambda e: e.memset(Cmb[:], 0.0), writes=["Cmb"])
                b_S = palloc()
                for h in range(4):
                    P.op("pe", lambda e, h=h, b=b_S: e.matmul(psf[b][:, h * 128:(h + 1) * 128], lhsT=ks[:, h, :], rhs=qs[:, h, :],
                                                              start=True, stop=True),
                         reads=["ks", "qs"], writes=["ps%d" % b_S])
                P.op("dve", lambda e, b=b_S: e.tensor_tensor(out=sm[:], in0=psf[b][:, :].rearrange("p (h t) -> p h t", h=4),
                                                             in1=maskb[:].unsqueeze(1).to_broadcast([128, 4, 128]), op=ALU.mult),
                     reads=["ps%d" % b_S, "maskb"], writes=["vmT0"])
                b_gt = 9
                if g == 0:
                    b_gb = palloc()
                    P.op("pe", lambda e, b=b_gb: e.matmul(psf[b][:, 0:8], lhsT=ones_row[0:1, :], rhs=gbias_row, start=True, stop=True),
                         reads=["ones_row", "rowsb"], writes=["ps%d" % b_gb])
                    P.op("dve", lambda e, b=b_gb: e.tensor_copy(out=gbb[:], in_=psf[b][:, 0:8]), reads=["ps%d" % b_gb], writes=["gbb"])
                srcs = [(qmT, ["qmT0", "qmT1"]), (kmT, ["kmT0", "kmT1"]), (vmT, ["oT0a", "oT0b"])]
                for c in range(24):
                    tsrc, tk = srcs[c // 8]
                    P.op("pe", lambda e, c=c, tsrc=tsrc, b=b_gt: e.matmul(psf[b][:, 0:8], lhsT=tsrc[:, c % 8, :], rhs=wg[:, c, :],
                                                                          start=(c == 0), stop=False),
                         reads=tk + ["wg"], writes=[PK(b_gt)])
                P.op("pe", lambda e, b=b_gt: e.matmul(psf[b][:, 0:8], lhsT=ident[:], rhs=gbb[:], start=False, stop=True),
                     reads=["ident", "gbb"], writes=[PK(b_gt)])
                P.op("act", lambda e, b=b_gt: e.activation(out=gtmp[:], in_=psf[b][:, 4:8], func=AF.Exp, scale=-1.0),
                     reads=[PK(b_gt)], writes=["gtmp"])
                P.op("act", lambda e: e.activation(out=gl[:, 0:4], in_=gtmp[:], func=AF.Ln, bias=1.0), reads=["gtmp"], writes=["gl0"])
                P.op("act", lambda e, b=b_gt: e.activation(out=gl[:, 4:8], in_=psf[b][:, 0:4], func=AF.Copy),
                     reads=[PK(b_gt)], writes=["gl1"])
                b_o = []
                for half in range(2):
                    b = palloc()
                    b_o.append(b)
                    for hh in range(2):
                        h = half * 2 + hh
                        P.op("pe", lambda e, h=h, hh=hh, b=b: e.matmul(psf[b][:, hh * 256:(hh + 1) * 256], lhsT=sm[:, h, :],
                                                                       rhs=vg[:, h * 256:(h + 1) * 256], start=True, stop=False),
                             reads=["vmT0", "vg0", "vg1"], writes=["ps%d" % b])
                        P.op("pe", lambda e, h=h, hh=hh, b=b: e.matmul(psf[b][:, hh * 256:(hh + 1) * 256], lhsT=qs[:, h, :],
                                                                       rhs=Sgb[:, h, :], start=False, stop=True),
                             reads=["qs", "Sgb"], writes=["ps%d" % b])
                for h in range(4):
                    b = b_o[h // 2]
                    hh = h % 2
                    P.op("act", lambda e, h=h, hh=hh, b=b: e.activation(out=junkh[h], in_=psf[b][:, hh * 256:(hh + 1) * 256],
                                                                        func=AF.Square, accum_out=st_g[:, h:h + 1]),
                         reads=["ps%d" % b], writes=["st_g0_%d" % h, "oT1_%d" % h])
                SG0 = ["st_g0_%d" % h for h in range(4)]
                P.op("act", lambda e: e.activation(out=st_g[:, 4:8], in_=st_g[:, 0:4], func=AF.Ln, scale=1.0 / 256, bias=EPS),
                     reads=SG0, writes=["st_g1"])
                P.op("act", lambda e: e.activation(out=st_g[:, 8:12], in_=st_g[:, 4:8], func=AF.Exp, scale=-0.5),
                     reads=["st_g1"], writes=["st_g2"])
                for h in range(4):
                    b = b_o[h // 2]
                    hh = h % 2
                    P.op("dve", lambda e, h=h, hh=hh, b=b: e.scalar_tensor_tensor(
                        out=obf[:, h * 256:(h + 1) * 256], in0=psf[b][:, hh * 256:(hh + 1) * 256], scalar=st_g[:, 8 + h:9 + h],
                        in1=szg[:, h * 256:(h + 1) * 256], op0=ALU.mult, op1=ALU.mult),
                        reads=["ps%d" % b, "st_g2", "szg0", "szg1"], writes=["obf_g%d" % h])
                for half in range(2):
                    b = palloc()
                    for hh in range(2):
                        h = half * 2 + hh
                        P.op("pe", lambda e, h=h, hh=hh, b=b: e.matmul(psf[b][:, hh * 256:(hh + 1) * 256], lhsT=kdec[:, h * 128:(h + 1) * 128],
                                                                       rhs=vg[:, h * 256:(h + 1) * 256], start=True, stop=True),
                             reads=["kdec", "vg0", "vg1"], writes=["ps%d" % b])
                    for hh in range(2):
                        h = half * 2 + hh
                        P.op("dve", lambda e, h=h, hh=hh, b=b: e.scalar_tensor_tensor(
                            out=Sg32[:, h, :], in0=Sg32[:, h, :], scalar=scrA[:, h * 128 + 127:h * 128 + 128],
                            in1=psf[b][:, hh * 256:(hh + 1) * 256], op0=ALU.mult, op1=ALU.add),
                            reads=["Sg32_%d" % h, "A0", "A1", "ps%d" % b], writes=["Sg32_%d" % h])

                b_kt2 = []
                for half in range(2):
                    b = palloc(avoid=b_kt2, hold=True)
                    b_kt2.append(b)
                    for cc in range(4):
                        c = half * 4 + cc
                        P.op("pe", lambda e, c=c, cc=cc, b=b: e.matmul(psf[b][:, cc * 128:(cc + 1) * 128], lhsT=cactT[:, c, :],
                                                                       rhs=Wk[:, c, :], start=True, stop=True),
                             reads=CK + ["Wk"], writes=["ps%d" % b])
                b_Sm = palloc(hold=True)
                for h in range(4):
                    for dc in range(2):
                        P.op("pe", lambda e, h=h, dc=dc, b=b_Sm: e.matmul(psf[b][:, h * 128:(h + 1) * 128], lhsT=kmT[:, 2 * h + dc, :],
                                                                          rhs=qmT[:, 2 * h + dc, :], start=(dc == 0), stop=(dc == 1)),
                             reads=["kmT0", "kmT1", "qmT0", "qmT1"], writes=["ps%d" % b_Sm])
                b_ga = 10
                ga = psf[b_ga]
                GL = ["gl0", "gl1"]
                P.op("pe", lambda e, ga=ga: e.matmul(ga[:, 0:4], lhsT=triN[:], rhs=gl[:, 0:4], start=True, stop=True),
                     reads=GL + ["triN"], writes=[PK(b_ga)])
                P.op("pe", lambda e, ga=ga: e.matmul(ga[:, 4:8], lhsT=onesN[:], rhs=gl[:, 0:4], start=True, stop=True),
                     reads=GL + ["onesN"], writes=[PK(b_ga)])
                P.op("pe", lambda e, ga=ga: e.matmul(ga[:, 8:12], lhsT=maskf[:], rhs=gl[:, 0:4], start=True, stop=False),
                     reads=GL + ["maskf"], writes=[PK(b_ga)])
                P.op("pe", lambda e, ga=ga: e.matmul(ga[:, 8:12], lhsT=identf[:], rhs=gl[:, 4:8], start=False, stop=True),
                     reads=GL + ["identf"], writes=[PK(b_ga)])
                P.op("pe", lambda e, ga=ga: e.matmul(ga[:, 12:16], lhsT=UN[:], rhs=gl[:, 0:4], start=True, stop=False),
                     reads=GL + ["UN"], writes=[PK(b_ga)])
                P.op("pe", lambda e, ga=ga: e.matmul(ga[:, 12:16], lhsT=identf[:], rhs=gl[:, 4:8], start=False, stop=True),
                     reads=GL + ["identf"], writes=[PK(b_ga)])
                P.op("act", lambda e, ga=ga: e.activation(out=gex[:, 0:8], in_=ga[:, 0:8], func=AF.Exp), reads=[PK(b_ga)], writes=["gex0"])
                P.op("act", lambda e, ga=ga: e.activation(out=gex[:, 8:16], in_=ga[:, 8:16], func=AF.Exp, bias=-LN16),
                     reads=[PK(b_ga)], writes=["gex1"])
                yield "MS"
                for half in range(2):
                    b = b_kt2[half]
                    for hh in range(2):
                        h = half * 2 + hh
                        P.op("dve", lambda e, h=h, hh=hh, b=b: e.tensor_scalar(
                            out=kw[:, h * 256:(h + 1) * 256], in0=psf[b][:, hh * 256:(hh + 1) * 256],
                            scalar1=gex[:, 12 + h:13 + h], scalar2=None, op0=ALU.mult),
                            reads=["ps%d" % b, "gex1"], writes=["kw%d" % h])
                prel(*b_kt2)
                for h in range(4):
                    P.op("dve", lambda e, h=h, b=b_Sm: e.scalar_tensor_tensor(
                        out=smm[:, h, :], in0=psf[b][:, h * 128:(h + 1) * 128], scalar=gex[:, 8 + h:9 + h], in1=maskb[:],
                        op0=ALU.mult, op1=ALU.mult),
                        reads=["ps%d" % b_Sm, "gex1", "maskb"], writes=["R%d" % h])
                prel(b_Sm)
                b_P2 = [palloc(hold=True)]
                b_P2.append(palloc(avoid=b_P2, hold=True))
                b_den = 11
                PH = lambda h: psf[b_P2[h // 2]][:, (h % 2) * 256:(h % 2 + 1) * 256]
                for h in range(4):
                    pk = "ps%d" % b_P2[h // 2]
                    P.op("pe", lambda e, h=h: e.matmul(PH(h), lhsT=smm[:, h, :], rhs=vaug[:, h, 0:256], start=True, stop=False),
                         reads=["R%d" % h, "vaug0", "vaug1"], writes=[pk])
                    for dc in range(2):
                        P.op("pe", lambda e, h=h, dc=dc: e.matmul(PH(h), lhsT=qmT[:, 2 * h + dc, :], rhs=Cmb[:, 2 * h + dc, 0:256],
                                                                  start=False, stop=(dc == 1)),
                             reads=["qmT0", "qmT1", "Cmb"], writes=[pk])
                for h in range(4):
                    dk = PK(b_den)
                    P.op("pe", lambda e, h=h: e.matmul(psf[b_den][:, 2 * h:2 * h + 2], lhsT=smm[:, h, :], rhs=vaug[:, h, 255:257], start=True, stop=False),
                         reads=["R%d" % h, "vaug0", "vaug1"], writes=[dk])
                    for dc in range(2):
                        P.op("pe", lambda e, h=h, dc=dc: e.matmul(psf[b_den][:, 2 * h:2 * h + 2], lhsT=qmT[:, 2 * h + dc, :], rhs=Cmb[:, 2 * h + dc, 255:257],
                                                                  start=False, stop=(dc == 1)),
                             reads=["qmT0", "qmT1", "Cmb"], writes=[dk])
                b_P = b_P2 + [b_den]
                for h in range(4):
                    for dc in range(2):
                        b = palloc(avoid=b_P)
                        P.op("pe", lambda e, h=h, dc=dc, b=b: e.matmul(psf[b][:, 0:257], lhsT=kw[:, h * 256 + dc * 128:h * 256 + (dc + 1) * 128],
                                                                       rhs=vaug[:, h, :], start=True, stop=True),
                             reads=["kw%d" % h, "vaug0", "vaug1"], writes=["ps%d" % b])
                        P.op("dve", lambda e, h=h, dc=dc, b=b: e.scalar_tensor_tensor(
                            out=Cm32[:, 2 * h + dc, :], in0=Cm32[:, 2 * h + dc, :], scalar=gex[:, 4 + h:5 + h],
                            in1=psf[b][:, 0:257], op0=ALU.mult, op1=ALU.add),
                            reads=["Cm32_%d" % (2 * h + dc), "gex0", "ps%d" % b], writes=["Cm32_%d" % (2 * h + dc)])

                yield "MP"
                for h in range(4):
                    P.op("act", lambda e, h=h: e.activation(out=junkh[h], in_=PH(h), func=AF.Square, accum_out=st_m[:, h:h + 1]),
                         reads=["ps%d" % b_P2[h // 2]], writes=["st_m_ssq%d" % h, "oT1_%d" % h])
                P.op("act", lambda e: e.activation(out=st_m[:, 4:8], in_=psf[b_den][:, 0:8].rearrange("p (h two) -> p h two", two=2)[:, :, 1], func=AF.Copy),
                     reads=[PK(b_den)], writes=["st_m_den"])
                SSQ = ["st_m_ssq%d" % h for h in range(4)]
                DEN = ["st_m_den"]
                P.op("dve", lambda e: e.tensor_tensor(out=st_m[:, 8:12], in0=st_m[:, 4:8], in1=gex[:, 0:4], op=ALU.mult),
                     reads=DEN + ["gex0"], writes=["st_m_D"])
                P.op("dve", lambda e: e.tensor_scalar(out=st_m[:, 12:16], in0=st_m[:, 8:12], scalar1=-1.0, scalar2=1.0,
                                                      op0=ALU.mult, op1=ALU.max),
                     reads=["st_m_D"], writes=["st_m_mx0"])
                P.op("dve", lambda e: e.tensor_tensor(out=st_m[:, 12:16], in0=st_m[:, 12:16], in1=st_m[:, 8:12], op=ALU.max),
                     reads=["st_m_D", "st_m_mx0"], writes=["st_m_mx"])
                P.op("dve", lambda e: e.reciprocal(out=st_m[:, 16:20], in_=st_m[:, 12:16]), reads=["st_m_mx"], writes=["st_m_ri"])
                P.op("dve", lambda e: e.tensor_tensor(out=st_m[:, 20:24], in0=st_m[:, 16:20], in1=gex[:, 0:4], op=ALU.mult),
                     reads=["st_m_ri", "gex0"], writes=["st_m_r"])
                P.op("dve", lambda e: e.tensor_tensor(out=st_m[:, 24:28], in0=st_m[:, 20:24], in1=st_m[:, 20:24], op=ALU.mult),
                     reads=["st_m_r"], writes=["st_m_v"])
                P.op("dve", lambda e: e.tensor_tensor(out=st_m[:, 24:28], in0=st_m[:, 24:28], in1=st_m[:, 0:4], op=ALU.mult),
                     reads=["st_m_v"] + SSQ, writes=["st_m_v"])
                P.op("act", lambda e: e.activation(out=st_m[:, 28:32], in_=st_m[:, 24:28], func=AF.Ln, scale=1.0 / 256, bias=EPS),
                     reads=["st_m_v"], writes=["st_m_ln"])
                P.op("act", lambda e: e.activation(out=st_m[:, 32:36], in_=st_m[:, 28:32], func=AF.Exp, scale=-0.5),
                     reads=["st_m_ln"], writes=["st_m_rs"])
                P.op("dve", lambda e: e.tensor_tensor(out=st_m[:, 36:40], in0=st_m[:, 32:36], in1=st_m[:, 20:24], op=ALU.mult),
                     reads=["st_m_rs", "st_m_r"], writes=["st_m_s"])
                t1 = scrA
                for h in range(4):
                    P.op("dve", lambda e, h=h: e.scalar_tensor_tensor(
                        out=t1[:, h * 256:(h + 1) * 256], in0=PH(h), scalar=st_m[:, 36 + h:37 + h],
                        in1=gmn[:, h * 256:(h + 1) * 256], op0=ALU.mult, op1=ALU.mult),
                        reads=["ps%d" % b_P2[h // 2], "st_m_s", "gmn"], writes=["A%d" % h])
                prel(*b_P2)
                for half in range(2):
                    b = palloc()
                    for cc in range(4):
                        c = half * 4 + cc
                        P.op("pe", lambda e, c=c, cc=cc, b=b: e.matmul(psf[b][:, cc * 128:(cc + 1) * 128], lhsT=cactT[:, c, :],
                                                                       rhs=dskip[:, c, :], start=True, stop=True),
                             reads=CK + ["dskip"], writes=["ps%d" % b])
                    P.op("dve", lambda e, half=half, b=b: e.tensor_tensor(out=t1[:, half * 512:(half + 1) * 512], in0=psf[b][:, :],
                                                                          in1=t1[:, half * 512:(half + 1) * 512], op=ALU.add),
                         reads=["ps%d" % b, "A%d" % (2 * half), "A%d" % (2 * half + 1)], writes=["A%d" % (2 * half), "A%d" % (2 * half + 1)])
                    P.op("dve", lambda e, half=half: e.tensor_tensor(out=obf[:, D + half * 512:D + (half + 1) * 512],
                                                                     in0=t1[:, half * 512:(half + 1) * 512],
                                                                     in1=szm[:, half * 512:(half + 1) * 512], op=ALU.mult),
                         reads=["A%d" % (2 * half), "A%d" % (2 * half + 1), "szm%d" % half], writes=["obf_m%d" % half])
                P.op("act", lambda e: e.activation(out=Sgb[:].rearrange("p h e -> p (h e)"), in_=Sg32[:].rearrange("p h e -> p (h e)"), func=AF.Copy),
                     reads=["Sg32_%d" % h for h in range(4)], writes=["Sgb"])
                P.op("act", lambda e: e.activation(out=dmy[:, 1:2], in_=dmy[:, 0:1], func=AF.Silu), reads=["dmy0"], writes=["dmy1"])
                yield "PP"
                if g == 0:
                    for c in range(8):
                        P.op("dve", lambda e, c=c: e.tensor_scalar(out=wout[:, c, :], in0=wout[:, c, :], scalar1=p128[:, 8 + c:9 + c],
                                                                    scalar2=None, op0=ALU.mult),
                             reads=["wout0", "p128"], writes=["wout0"])
                OB = ["obf_g%d" % h for h in range(4)] + ["obf_m0", "obf_m1"]
                for half in range(2):
                    b = palloc()
                    for cc in range(8):
                        c = half * 8 + cc
                        P.op("pe", lambda e, c=c, cc=cc, b=b: e.transpose(out=psb[b][:, cc * 128:(cc + 1) * 128],
                                                                          in_=obf[:, c * 128:(c + 1) * 128], identity=ident[:]),
                             reads=(OB[0:4] if half == 0 else OB[4:6]) + ["ident"], writes=["ps%d" % b])
                    if half == 0:
                        P.op("act", lambda e, b=b: e.activation(out=oT[:, 0:8, :], in_=psb[b][:, 0:1024].rearrange("p (c t) -> p c t", c=8),
                                                                func=AF.Copy),
                             reads=["ps%d" % b], writes=["oT0a", "oT0b"])
                    else:
                        P.op("dve", lambda e, b=b: e.tensor_copy(out=oT[:, 8:16, :], in_=psb[b][:, 0:1024].rearrange("p (c t) -> p c t", c=8)),
                             reads=["ps%d" % b], writes=OTK[1:])
                P.op("dve", lambda e: e.tensor_copy(out=Cmb[:].rearrange("p h e -> p (h e)"), in_=Cm32[:].rearrange("p h e -> p (h e)")),
                     reads=["Cm32_%d" % c for c in range(8)], writes=["Cmb"])
                P.op("act", lambda e: e.activation(out=dmy[:, 3:4], in_=dmy[:, 2:3], func=AF.Exp), reads=["dmy2"], writes=["dmy3"])
                yield "FT"
                P.op("sp", lambda e, g=g: e.dma_start(out=yt[:], in_=x_d[g * 128:(g + 1) * 128, :]), writes=OB, chan="xr")
                b_y = [palloc()]
                b_y.append(palloc(avoid=b_y))
                for kk in range(2):
                    for half in range(2):
                        b = b_y[half]
                        for c in range(kk * 8, kk * 8 + 8):
                            P.op("pe", lambda e, c=c, half=half, b=b: e.matmul(psf[b][:, :], lhsT=oT[:, c, :],
                                                                               rhs=wout[:, c, half * 512:(half + 1) * 512],
                                                                               start=(c == 0), stop=(c == 15)),
                                 reads=((["oT0a"] if c < 4 else ["oT0b"]) + ["wout0"] if c < 8 else OTK[1:] + ["wout1"]), writes=["ps%d" % b])
                for half in range(2):
                    b = b_y[half]
                    P.op("dve", lambda e, half=half, b=b: e.tensor_tensor(out=yt[:, half * 512:(half + 1) * 512], in0=psf[b][:, :],
                                                                          in1=yt[:, half * 512:(half + 1) * 512], op=ALU.add),
                         reads=["ps%d" % b] + (OB[0:4] if half == 0 else OB[4:6]), writes=(OB[0:4] if half == 0 else OB[4:6]))
                yield "F"
                P.op("act", lambda e: e.activation(out=junk, in_=yt[:], func=AF.Square, accum_out=st_y[:, 0:1]),
                     reads=OB, writes=["st_y0", "oT0a", "oT0b"])
                P.op("act", lambda e: e.activation(out=st_y[:, 1:2], in_=st_y[:, 0:1], func=AF.Ln, scale=1.0 / D, bias=EPS),
                     reads=["st_y0"], writes=["st_y1"])
                P.op("act", lambda e: e.activation(out=st_y[:, 2:3], in_=st_y[:, 1:2], func=AF.Exp, scale=-0.5),
                     reads=["st_y1"], writes=["st_y2"])
                P.op("dve", lambda e: e.scalar_tensor_tensor(out=yt[:], in0=yt[:], scalar=st_y[:, 2:3], in1=gfin[:], op0=ALU.mult, op1=ALU.mult),
                     reads=OB + ["st_y2", "gfin"], writes=OB)
                ok = "out%d" % g
                out_keys.append(ok)
                P.op("sp", lambda e, g=g: e.dma_start(out=out_d[g * 128:(g + 1) * 128, :], in_=yt[:]),
                     reads=OB, writes=[ok], chan="st")

        tiles = [(s * ntile + ti, s, ti) for s in range(nseq) for ti in range(ntile)]
        gens = [tile(*t) for t in tiles]

        def run(gi, upto):
            if gi >= len(gens):
                return
            for tag in gens[gi]:
                if tag == upto:
                    return
            assert upto is None, (gi, upto)

        run(0, "A1"); run(0, "A"); init_diag([2, 3], True); run(0, "B1"); run(0, "CV"); run(0, "Z"); run(0, "QP"); run(1, "A1")
        for n in range(len(gens)):
            run(n, "TM")
            run(n + 1, "A")
            run(n, "MS")
            run(n + 1, "B1")
            run(n, "MP")
            run(n, "PP")
            run(n + 1, "CV")
            run(n + 1, "Z")
            run(n, "FT")
            run(n + 1, "QP")
            run(n + 2, "A1")
            run(n, None)

        P.op("sp", lambda e: e.nop(), reads=out_keys)
        P.emit()
    return nc


def prep_shared(norm_g, w_in, w_gla_gate_up, b_gla_gate, gla_norm_g, conv_w, conv_b,
                w_q_m, w_k_m, w_v_m, w_igate, b_igate, w_fgate, b_fgate,
                mlstm_norm_g, mlstm_skip, w_out, final_norm_g):
    f = lambda a: np.ascontiguousarray(np.asarray(a, dtype=np.float32))
    pp = lambda v: f(v).reshape(8, 128).T
    p128 = np.concatenate([pp(norm_g), pp(f(gla_norm_g).reshape(-1)), pp(mlstm_skip)]
                          + [pp(f(conv_w)[k]) for k in range(4)] + [pp(conv_b)], axis=1)
    rows = np.concatenate([f(conv_b).reshape(1, -1), f(b_igate).reshape(1, -1), f(b_fgate).reshape(1, -1)], axis=1)
    wup = np.concatenate([f(w_gla_gate_up), f(b_gla_gate).reshape(1, -1), np.zeros((111, 512), np.float32)], axis=0)

    def expand(w):
        w = f(w)
        o = np.zeros((1024, 128), np.float32)
        for n in range(256):
            c, nl = divmod(n, 32)
            o[c * 128 + 4 * nl:c * 128 + 4 * nl + 4, 4 * nl:4 * nl + 4] = w[n]
        return o

    wg = np.concatenate([f(w_igate), f(w_fgate)], axis=1)
    wg = wg.reshape(24, 128, 8).transpose(1, 0, 2).reshape(128, 192)
    return {
        "w_in": f(w_in), "w_out": f(w_out), "p128": f(p128), "rows": f(rows), "wup": f(wup),
        "wq": expand(w_q_m), "wk": expand(w_k_m), "wv": expand(w_v_m), "wg": f(wg),
        "gmn": f(mlstm_norm_g).reshape(1, -1), "gfin": f(final_norm_g).reshape(1, -1),
    }


_NC_CACHE = {}


def kernel(x, **params):
    x = np.asarray(x, dtype=np.float32)
    bsz, seq, _ = x.shape
    nseq = bsz // NCORES
    ntile = seq // 128
    key = (nseq, ntile)
    if key not in _NC_CACHE:
        _NC_CACHE[key] = build(nseq, ntile)
    nc = _NC_CACHE[key]
    shared = prep_shared(**params)
    in_maps = []
    for c in range(NCORES):
        m = dict(shared)
        m["x"] = np.ascontiguousarray(x[c * nseq:(c + 1) * nseq].reshape(nseq * seq, D))
        in_maps.append(m)
    res = run_bass_kernel_spmd(nc, in_maps, core_ids=list(range(NCORES)))
    outs = [np.asarray(r["out"]).reshape(nseq, seq, D) for r in res.results]
    return np.concatenate(outs, axis=0).astype(np.float32)
```

```python
import contextlib
import math
import numpy as np
import concourse.bass as bass
import concourse.mybir as mybir
from concourse.bass_utils import run_bass_kernel_spmd

F32 = mybir.dt.float32
BF16 = mybir.dt.bfloat16
AF = mybir.ActivationFunctionType
ALU = mybir.AluOpType

D = 1024
PW = 5136
NCORES = 8
ENGS = ("pe", "act", "dve", "pool", "sp")
EPS = 1e-6
LN16 = math.log(16.0)


class Prog:
    def __init__(self, nc):
        self.nc = nc
        self.ops = []
        self.last_use = {}

    def op(self, eng, fn, reads=(), writes=(), chan=None):
        for k in tuple(reads) + tuple(writes):
            if k.startswith("ps"):
                self.last_use[k] = len(self.ops)
        self.ops.append((eng, fn, tuple(reads), tuple(writes), chan))

    def emit(self):
        nc, ops = self.nc, self.ops
        n = len(ops)
        deps = [set() for _ in range(n)]
        last_w, readers = {}, {}
        needs_inc = [False] * n
        for i, (eng, fn, reads, writes, chan) in enumerate(ops):
            for k in reads:
                if k in last_w:
                    deps[i].add(last_w[k])
            for k in writes:
                if k in last_w:
                    deps[i].add(last_w[k])
                for r in readers.get(k, ()):
                    if r != i:
                        deps[i].add(r)
            for k in reads:
                readers.setdefault(k, []).append(i)
            for k in writes:
                last_w[k] = i
                readers[k] = []

        def semkey(i):
            eng, _, _, _, chan = ops[i]
            return ("D", chan) if chan is not None else ("E", eng)

        for i in range(n):
            eng = ops[i][0]
            rm = set()
            for j in deps[i]:
                if ops[j][4] is None and ops[i][4] is None and ops[j][0] == "pe" and eng == "pe":
                    rm.add(j)
            deps[i] -= rm
            last = {}
            for j in deps[i]:
                sk = semkey(j)
                if sk not in last or j > last[sk]:
                    last[sk] = j
            deps[i] = set(last.values())
            for j in deps[i]:
                needs_inc[j] = True
        for i in range(n):
            if ops[i][4] is not None:
                needs_inc[i] = True
        counts, ticket = {}, [None] * n
        for i in range(n):
            if needs_inc[i]:
                sk = semkey(i)
                counts[sk] = counts.get(sk, 0) + (16 if sk[0] == "D" else 1)
                ticket[i] = (sk, counts[sk])
        with contextlib.ExitStack() as st:
            sems = {sk: st.enter_context(nc.semaphore("s_%s_%s" % sk)) for sk in sorted(counts)}
            block = st.enter_context(nc.Block())
            handles = {"pe": block.tensor, "act": block.scalar, "dve": block.vector,
                       "pool": block.gpsimd, "sp": block.sync}
            for eng in ENGS:
                my = [i for i in range(n) if ops[i][0] == eng]
                if not my:
                    continue

                def body(e, my=my):
                    waited = {}
                    for i in my:
                        for j in sorted(deps[i]):
                            sk, v = ticket[j]
                            if waited.get(sk, 0) < v:
                                e.wait_ge(sems[sk], v)
                                waited[sk] = v
                        ins = ops[i][1](e)
                        if needs_inc[i]:
                            sk, v = ticket[i]
                            ins.then_inc(sems[sk], 16 if sk[0] == "D" else 1)

                handles[eng](body)


def build(nseq=2, ntile=16):
    ntok = nseq * ntile * 128
    nc = bass.Bass("TRN2", target_bir_lowering=False)
    dram = lambda name, shape, kind="ExternalInput": nc.dram_tensor(name, shape, F32, kind=kind).ap()
    x_d = dram("x", [ntok, D])
    win_d = dram("w_in", [D, PW])
    wout_d = dram("w_out", [2 * D, D])
    p128_d = dram("p128", [128, 64])
    rows_d = dram("rows", [1, 1032])
    wup_d = dram("wup", [128, 512])
    wq_d = dram("wq", [D, 128])
    wk_d = dram("wk", [D, 128])
    wv_d = dram("wv", [D, 128])
    wg_d = dram("wg", [128, 192])
    gmn_d = dram("gmn", [1, D])
    gfin_d = dram("gfin", [1, D])
    out_d = dram("out", [ntok, D], kind="ExternalOutput")

    st = contextlib.ExitStack()
    with st:
        sb = lambda name, shape, dt: st.enter_context(nc.sbuf_tensor(name, shape, dt))
        P = Prog(nc)
        win = sb("win", [128, 8, PW], BF16)
        wout = sb("wout", [128, 16, D], BF16)
        Wq = sb("Wq", [128, 8, 128], BF16)
        Wk = sb("Wk", [128, 8, 128], BF16)
        Wv = sb("Wv", [128, 8, 128], BF16)
        dconv = sb("dconv", [128, 4, 8, 128], BF16)
        dskip = sb("dskip", [128, 8, 128], BF16)
        wg = sb("wgt", [128, 24, 8], BF16)
        gmn = sb("gmn_bc", [128, D], F32)
        gfin = sb("gfin_bc", [128, D], F32)
        p128 = sb("p128t", [128, 64], F32)
        rowsb = sb("rowsb", [1, 1032], BF16)
        wup = sb("wupt", [128, 512], BF16)
        identf = sb("identf", [128, 128], F32)
        ident = sb("ident", [128, 128], BF16)
        maskf = sb("maskf", [128, 128], F32)
        maskb = sb("maskb", [128, 128], BF16)
        triG = sb("triG", [128, 128], BF16)
        UG = sb("UG", [128, 128], BF16)
        triN = sb("triN", [128, 128], F32)
        UN = sb("UN", [128, 128], F32)
        onesN = sb("onesN", [128, 128], F32)
        ones_row = sb("ones_row", [1, 128], BF16)
        raug = sb("raug", [128, 128], BF16)
        gbb = sb("gbb", [128, 8], BF16)
        Sg32 = sb("Sg32", [128, 4, 256], F32)
        Sgb = sb("Sgb", [128, 4, 256], BF16)
        Cm32 = sb("Cm32", [128, 8, 257], F32)
        Cmb = sb("Cmb", [128, 8, 257], BF16)
        xt = sb("xt", [128, D], F32)
        xs = sb("xs", [128, D], BF16)
        xT = sb("xT", [128, 8, 128], BF16)
        xme = sb("xme", [128, 8, 131], BF16)
        vg = sb("vg", [128, D], BF16)
        szg = sb("szg", [128, D], BF16)
        szm = sb("szm", [128, D], BF16)
        scrA = sb("scrA", [128, 1024], F32)
        scrB = sb("scrB", [128, 512], F32)
        qs = sb("qs", [128, 4, 128], BF16)
        ks = sb("ks", [128, 4, 128], BF16)
        kdec = sb("kdec", [128, 512], BF16)
        spb = kdec
        cactT = sb("cactT", [128, 8, 128], BF16)
        qmT = sb("qmT", [128, 8, 128], BF16)
        kmT = sb("kmT", [128, 8, 128], BF16)
        smb = sb("smb", [128, 8, 128], BF16)
        sm = smb[:, 0:4, :]
        smm = smb[:, 4:8, :]
        vaug = sb("vaug", [128, 4, 257], BF16)
        kw = sb("kw", [128, D], BF16)
        obf = sb("obf", [128, 2 * D], BF16)
        oT = sb("oT", [128, 16, 128], BF16)
        yt = obf[:, :].bitcast(F32)
        junk = oT[:, 0:8, :].rearrange("p c t -> p (c t)")
        vmT = oT[:, 0:8, :]
        junkh = [oT[:, 8 + 2 * h:10 + 2 * h, :].rearrange("p c t -> p (c t)") for h in range(4)]
        OTK = ["oT0", "oT1_0", "oT1_1", "oT1_2", "oT1_3"]
        st_x = sb("st_x", [128, 4], F32)
        st_g = sb("st_g", [128, 12], F32)
        gl = sb("gl", [128, 8], F32)
        gtmp = sb("gtmp", [128, 4], F32)
        gex = sb("gex", [128, 20], F32)
        st_m = sb("st_m", [128, 40], F32)
        st_y = sb("st_y", [128, 4], F32)
        dmy = sb("dmy", [128, 4], F32)

        psf, psb = [], []
        for b in range(8):
            t = st.enter_context(nc.psum_tensor("ps%d" % b, [128, 512], F32))
            psf.append(t)
            psb.append(t.bitcast(BF16))
        psf += [psf[7][:, 0:128], psf[7][:, 128:144], psf[7][:, 144:160], psf[7][:, 160:168]]
        pctr = [0]
        PK = lambda b: "ps%d" % min(b, 7)

        held = set()

        def palloc(avoid=(), hold=False):
            cands = [b for b in range(7) if b not in avoid and b not in held]
            assert cands, "out of PSUM banks"
            b = min(cands, key=lambda b: P.last_use.get("ps%d" % b, -1 - (8 - b)))
            P.last_use["ps%d" % b] = len(P.ops)
            if hold:
                held.add(b)
            return b

        def prel(*banks):
            for b in banks:
                held.discard(b)

        P.op("pool", lambda e: e.memset(identf[:], 0.0), writes=["identf"])
        P.op("pool", lambda e: e.affine_select(out=identf[:], in_=identf[:], pattern=[[-1, 128]],
                                               compare_op=ALU.not_equal, fill=1.0, base=0, channel_multiplier=1),
             reads=["identf"], writes=["identf"])
        P.op("pool", lambda e: e.memset(maskf[:], 1.0), writes=["maskf"])
        P.op("pool", lambda e: e.affine_select(out=maskf[:], in_=maskf[:], pattern=[[1, 128]],
                                               compare_op=ALU.is_ge, fill=0.0, base=0, channel_multiplier=-1),
             reads=["maskf"], writes=["maskf"])
        WGRP = [(3072, 4112), (2048, 3072), (4112, 5136), (0, 1024), (1024, 2048)]

        def load_wgroup(gi):
            c0, c1 = WGRP[gi]
            for k in range(8):
                P.op("pool", lambda e, k=k, c0=c0, c1=c1: e.dma_start(out=win[:, k, c0:c1], in_=win_d[k * 128:(k + 1) * 128, c0:c1],
                                                                      max_dma_last_dim=4096),
                     writes=["win%d_%d" % (gi, k)], chan="win%d_%d" % (gi, k))

        def wkey(col0, k):
            for gi, (c0, c1) in enumerate(WGRP):
                if c0 <= col0 < c1:
                    return "win%d_%d" % (gi, k)
            raise KeyError(col0)
        P.op("sp", lambda e: e.dma_start(out=p128[:], in_=p128_d), writes=["p128"], chan="p128")
        P.op("pool", lambda e: e.dma_start(out=wup[:], in_=wup_d), writes=["wup"], chan="wup")
        load_wgroup(0)
        P.op("pool", lambda e: e.dma_start(out=rowsb[:], in_=rows_d), writes=["rowsb"], chan="rows")
        P.op("sp", lambda e: e.dma_start(out=gmn[:], in_=gmn_d.partition_broadcast(128)), writes=["gmn"], chan="gmn")
        P.op("sp", lambda e: e.dma_start(out=gfin[:], in_=gfin_d.partition_broadcast(128)), writes=["gfin"], chan="gfin")
        load_wgroup(1)
        load_wgroup(2)
        load_wgroup(3)
        P.op("pool", lambda e: e.dma_start(out=Wq[:], in_=wq_d.rearrange("(c p) n -> p c n", p=128)), writes=["Wq"], chan="Wq")
        P.op("pool", lambda e: e.dma_start(out=Wk[:], in_=wk_d.rearrange("(c p) n -> p c n", p=128)), writes=["Wk"], chan="Wk")
        P.op("pool", lambda e: e.dma_start(out=Wv[:], in_=wv_d.rearrange("(c p) n -> p c n", p=128)), writes=["Wv"], chan="Wv")
        P.op("pool", lambda e: e.dma_start(out=wg[:], in_=wg_d.rearrange("p (c j) -> p c j", j=8)), writes=["wg"], chan="wg")
        load_wgroup(4)
        for kk in range(2):
            P.op("pool", lambda e, kk=kk: e.dma_start(
                out=wout[:, kk * 8:(kk + 1) * 8, :],
                in_=wout_d[kk * 1024:(kk + 1) * 1024, :].rearrange("(c p) n -> p c n", p=128)),
                writes=["wout%d" % kk], chan="wout%d" % kk)

        P.op("dve", lambda e: e.tensor_copy(out=ident[:], in_=identf[:]), reads=["identf"], writes=["ident"])
        P.op("dve", lambda e: e.tensor_copy(out=maskb[:], in_=maskf[:]), reads=["maskf"], writes=["maskb"])
        P.op("dve", lambda e: e.tensor_scalar(out=triG[:], in0=maskf[:], scalar1=-1.0 / 16, scalar2=None, op0=ALU.mult),
             reads=["maskf"], writes=["triG"])
        P.op("dve", lambda e: e.tensor_scalar(out=UG[:], in0=maskf[:], scalar1=-1.0, scalar2=1.0 / 16, op0=ALU.add, op1=ALU.mult),
             reads=["maskf"], writes=["UG"])
        P.op("dve", lambda e: e.tensor_scalar(out=triN[:], in0=maskf[:], scalar1=-1.0, scalar2=None, op0=ALU.mult),
             reads=["maskf"], writes=["triN"])
        P.op("dve", lambda e: e.tensor_scalar(out=UN[:], in0=maskf[:], scalar1=-1.0, scalar2=None, op0=ALU.add),
             reads=["maskf"], writes=["UN"])
        P.op("dve", lambda e: e.memset(onesN[:], -1.0), writes=["onesN"])
        P.op("dve", lambda e: e.memset(dmy[:], 0.0), writes=["dmy0", "dmy1", "dmy2", "dmy3"])
        P.op("dve", lambda e: e.memset(ones_row[:], 1.0), writes=["ones_row"])
        P.op("dve", lambda e: e.memset(raug[:], 1.0), writes=["raug"])
        P.op("dve", lambda e: e.memset(vaug[:], 1.0), writes=["vaug"])
        for k in range(4):
            for c in range(8):
                P.op("dve", lambda e, k=k, c=c: e.tensor_scalar(out=dconv[:, k, c, :], in0=identf[:],
                                                                 scalar1=p128[:, 24 + k * 8 + c:25 + k * 8 + c],
                                                                 scalar2=None, op0=ALU.mult),
                     reads=["identf", "p128"], writes=["dconv"])
        for c in range(8):
            P.op("dve", lambda e, c=c: e.tensor_scalar(out=dskip[:, c, :], in0=identf[:], scalar1=p128[:, 16 + c:17 + c],
                                                        scalar2=None, op0=ALU.mult),
                 reads=["identf", "p128"], writes=["dskip"])

        gT = p128[:, 0:8]
        convb_row = rowsb[0:1, 0:1024]
        gbias_row = rowsb[0:1, 1024:1032]

        def load_x(g):
            P.op("sp", lambda e, g=g: e.dma_start(out=xt[:], in_=x_d[g * 128:(g + 1) * 128, :]),
                 writes=["xt"], chan="xt")

        load_x(0)
        out_keys = []
        def tile(g, s, ti):
            if True:
                first = (ti == 0)
                P.op("act", lambda e: e.activation(out=xs[:], in_=xt[:], func=AF.Square, accum_out=st_x[:, 0:1]),
                     reads=["xt"], writes=["st_x0", "xs"])
                P.op("act", lambda e: e.activation(out=st_x[:, 1:2], in_=st_x[:, 0:1], func=AF.Ln, scale=1.0 / D, bias=EPS),
                     reads=["st_x0"], writes=["st_x1"])
                P.op("act", lambda e: e.activation(out=st_x[:, 2:3], in_=st_x[:, 1:2], func=AF.Exp, scale=-0.5),
                     reads=["st_x1"], writes=["st_x2"])
                P.op("dve", lambda e: e.tensor_scalar(out=xs[:], in0=xt[:], scalar1=st_x[:, 2:3], scalar2=None, op0=ALU.mult),
                     reads=["xt", "st_x2"], writes=["xs"])
                if g + 1 < nseq * ntile:
                    load_x(g + 1)
                yield "A1"
                bT_ = palloc()
                for c in range(8):
                    P.op("pe", lambda e, c=c, b=bT_: e.transpose(out=psb[b][:, c * 128:(c + 1) * 128],
                                                                  in_=xs[:, c * 128:(c + 1) * 128], identity=ident[:]),
                         reads=["xs", "ident"], writes=["ps%d" % bT_])
                P.op("dve", lambda e, b=bT_: e.tensor_tensor(
                    out=xT[:], in0=psb[b][:, 0:1024].rearrange("p (c t) -> p c t", c=8),
                    in1=gT.unsqueeze(2).to_broadcast([128, 8, 128]), op=ALU.mult),
                    reads=["ps%d" % bT_, "p128"], writes=["xT"])

                yield "A"
                def proj_fm(col0, nch, M=128, bank=None, hold=False):
                    b = palloc(hold=hold) if bank is None else bank
                    for ch in range(nch):
                        for k in range(8):
                            P.op("pe", lambda e, ch=ch, k=k, b=b: e.matmul(
                                psf[b][0:M, ch * 128:(ch + 1) * 128],
                                lhsT=win[:, k, col0 + ch * M: col0 + (ch + 1) * M], rhs=xT[:, k, :],
                                start=(k == 0), stop=(k == 7)),
                                reads=["xT", wkey(col0, k)], writes=[PK(b)])
                    return b

                b_r = proj_fm(3072, 1, M=128, bank=8)
                P.op("dve", lambda e, b=b_r: e.tensor_copy(out=raug[0:16, :], in_=psf[b][0:16, 0:128]),
                     reads=[PK(b_r)], writes=["raug"])
                if first:
                    P.op("pool", lambda e: e.memset(xme[:, :, 0:3], 0.0), writes=["xme_h"])
                else:
                    P.op("pool", lambda e: e.tensor_copy(out=xme[:, :, 0:3], in_=xme[:, :, 128:131]),
                         reads=["xme0", "xme1"], writes=["xme_h"])
                b_x0 = proj_fm(3088, 4)
                b_x1 = proj_fm(3088 + 512, 4)
                P.op("act", lambda e, b=b_x0: e.activation(out=xme[:, 0:4, 3:131], in_=psf[b][:, :].rearrange("p (c t) -> p c t", c=4),
                                                           func=AF.Copy),
                     reads=["ps%d" % b_x0, "xme_h"], writes=["xme0"])
                P.op("act", lambda e, b=b_x1: e.activation(out=xme[:, 4:8, 3:131], in_=psf[b][:, :].rearrange("p (c t) -> p c t", c=4),
                                                           func=AF.Copy),
                     reads=["ps%d" % b_x1, "xme_h"], writes=["xme1"])
                b_pre = palloc()
                P.op("pe", lambda e, b=b_pre: e.matmul(psf[b][:, :], lhsT=raug[:], rhs=wup[:], start=True, stop=True),
                     reads=["raug", "wup"], writes=["ps%d" % b_pre])
                P.op("act", lambda e, b=b_pre: e.activation(out=scrB[:], in_=psf[b][:, :], func=AF.Exp, scale=-1.0),
                     reads=["ps%d" % b_pre], writes=["scrB"])
                P.op("act", lambda e: e.activation(out=spb[:], in_=scrB[:], func=AF.Ln, bias=1.0),
                     reads=["scrB"], writes=["kdec"])
                yield "B1"
                b_cv = []
                for half in range(2):
                    b = palloc()
                    b_cv.append(b)
                    for cc in range(4):
                        c = half * 4 + cc
                        for k in range(4):
                            P.op("pe", lambda e, c=c, cc=cc, k=k, b=b: e.matmul(
                                psf[b][:, cc * 128:(cc + 1) * 128], lhsT=dconv[:, k, c, :], rhs=xme[:, c, k:k + 128],
                                start=(k == 0), stop=(k == 3)),
                                reads=["xme%d" % (c // 4), "xme_h", "dconv"], writes=["ps%d" % b])
                for c in range(8):
                    P.op("act", lambda e, c=c, b=b_cv[c // 4]: e.activation(
                        out=cactT[:, c, :], in_=psf[b][:, (c % 4) * 128:(c % 4 + 1) * 128], func=AF.Silu, bias=p128[:, 56 + c:57 + c]),
                        reads=["ps%d" % b_cv[c // 4], "p128"], writes=["cactT%d" % c])
                yield "CV"
                def proj_tm(col0):
                    b = palloc()
                    for k in range(8):
                        P.op("pe", lambda e, k=k, b=b: e.matmul(psf[b][:, :], lhsT=xT[:, k, :], rhs=win[:, k, col0:col0 + 512],
                                                                start=(k == 0), stop=(k == 7)),
                             reads=["xT", wkey(col0, k)], writes=["ps%d" % b])
                    return b

                for half in range(2):
                    b = proj_tm(2048 + half * 512)
                    P.op("act", lambda e, b=b, half=half: e.activation(out=szg[:, half * 512:(half + 1) * 512], in_=psf[b][:, :], func=AF.Silu),
                         reads=["ps%d" % b], writes=["szg%d" % half])
                for half in range(2):
                    b = proj_tm(4112 + half * 512)
                    P.op("act", lambda e, b=b, half=half: e.activation(out=szm[:, half * 512:(half + 1) * 512], in_=psf[b][:, :], func=AF.Silu),
                         reads=["ps%d" % b], writes=["szm%d" % half])
                yield "Z"
                b_q = proj_fm(0, 4, hold=True)
                b_k = proj_fm(512, 4, hold=True)
                yield "QP"
                b_bT = palloc()
                for h in range(4):
                    P.op("pe", lambda e, h=h, b=b_bT: e.matmul(psf[b][:, h * 128:(h + 1) * 128], lhsT=spb[:, h * 128:(h + 1) * 128],
                                                               rhs=triG[:], start=True, stop=True),
                         reads=["kdec", "triG"], writes=["ps%d" % b_bT])
                b_bL = palloc()
                P.op("pe", lambda e, b=b_bL: e.matmul(psf[b][:, :], lhsT=UG[:], rhs=spb[:], start=True, stop=True),
                     reads=["kdec", "UG"], writes=["ps%d" % b_bL])
                Eq = scrA[:, 0:512]
                Ek = scrA[:, 512:1024]
                P.op("act", lambda e, b=b_bT: e.activation(out=Eq, in_=psf[b][:, :], func=AF.Exp), reads=["ps%d" % b_bT], writes=["A0", "A1"])
                P.op("act", lambda e, b=b_bT: e.activation(out=Ek, in_=psf[b][:, :], func=AF.Exp, scale=-1.0),
                     reads=["ps%d" % b_bT], writes=["A2", "A3"])
                P.op("act", lambda e, b=b_bL: e.activation(out=scrB[:], in_=psf[b][:, :], func=AF.Exp),
                     reads=["ps%d" % b_bL], writes=["scrB"])
                P.op("dve", lambda e, b=b_q: e.scalar_tensor_tensor(out=qs[:].rearrange("p h t -> p (h t)"), in0=psf[b][:, :],
                                                                   scalar=128.0 ** -0.5, in1=Eq, op0=ALU.mult, op1=ALU.mult),
                     reads=["ps%d" % b_q, "A0", "A1"], writes=["qs"])
                P.op("dve", lambda e, b=b_k: e.tensor_tensor(out=ks[:].rearrange("p h t -> p (h t)"), in0=psf[b][:, :], in1=Ek, op=ALU.mult),
                     reads=["ps%d" % b_k, "A2", "A3"], writes=["ks"])
                prel(b_q, b_k)
                yield "C4"
                b_kt = proj_tm(512)
                P.op("dve", lambda e, b=b_kt: e.tensor_tensor(out=kdec[:], in0=psf[b][:, :], in1=scrB[:], op=ALU.mult),
                     reads=["ps%d" % b_kt, "scrB"], writes=["kdec"])
                for half in range(2):
                    b = proj_tm(1024 + half * 512)
                    if half == 0:
                        P.op("act", lambda e, b=b, half=half: e.activation(out=vg[:, half * 512:(half + 1) * 512], in_=psf[b][:, :], func=AF.Copy),
                             reads=["ps%d" % b], writes=["vg%d" % half])
                    else:
                        P.op("dve", lambda e, b=b, half=half: e.tensor_copy(out=vg[:, half * 512:(half + 1) * 512], in_=psf[b][:, :]),
                             reads=["ps%d" % b], writes=["vg%d" % half])
                yield "TM"
                def headwise_fm(Wt, wkey, src, skeys, dst, dkeys, eng="act"):
                    for half in range(2):
                        b = palloc()
                        for cc in range(4):
                            c = half * 4 + cc
                            P.op("pe", lambda e, c=c, cc=cc, b=b: e.matmul(psf[b][:, cc * 128:(cc + 1) * 128], lhsT=Wt[:, c, :],
                                                                           rhs=src(c), start=True, stop=True),
                                 reads=[wkey] + skeys, writes=["ps%d" % b])
                        if eng == "act":
                            P.op("act", lambda e, half=half, b=b: e.activation(
                                out=dst[:, half * 4:(half + 1) * 4, :], in_=psf[b][:, :].rearrange("p (c t) -> p c t", c=4), func=AF.Copy),
                                reads=["ps%d" % b], writes=dkeys(half))
                        else:
                            P.op("dve", lambda e, half=half, b=b: e.tensor_copy(
                                out=dst[:, half * 4:(half + 1) * 4, :], in_=psf[b][:, :].rearrange("p (c t) -> p c t", c=4)),
                                reads=["ps%d" % b], writes=dkeys(half))

                CK = ["cactT%d" % c for c in range(8)]
                headwise_fm(Wq, "Wq", lambda c: cactT[:, c, :], CK, qmT, lambda half: ["qmT%d" % half])
                headwise_fm(Wk, "Wk", lambda c: cactT[:, c, :], CK, kmT, lambda half: ["kmT%d" % half], eng="dve")
                RK = ["R0", "R1", "R2", "R3"]
                headwise_fm(Wv, "Wv", lambda c: xme[:, c, 3:131], ["xme0", "xme1"], vmT, lambda half: ["oT0a"] if half == 0 else ["oT0b"])
                for half in range(2):
                    b = palloc()
                    for cc in range(4):
                        c = half * 4 + cc
                        P.op("pe", lambda e, c=c, cc=cc, b=b: e.matmul(psf[b][:, cc * 128:(cc + 1) * 128], lhsT=xme[:, c, 3:131],
                                                                       rhs=Wv[:, c, :], start=True, stop=True),
                             reads=["xme%d" % (c // 4), "Wv"], writes=["ps%d" % b])
                    if half == 0:
                        P.op("act", lambda e, half=half, b=b: e.activation(
                            out=vaug[:, half * 2:(half + 1) * 2, 0:256], in_=psf[b][:, :].rearrange("p (h e) -> p h e", h=2), func=AF.Copy),
                            reads=["ps%d" % b], writes=["vaug%d" % half])
                    else:
                        P.op("dve", lambda e, half=half, b=b: e.tensor_copy(
                            out=vaug[:, half * 2:(half + 1) * 2, 0:256], in_=psf[b][:, :].rearrange("p (h e) -> p h e", h=2)),
                            reads=["ps%d" % b], writes=["vaug%d" % half])
                if first:
                    P.op("pool", lambda e: e.memset(Sg32[:], 0.0), writes=["Sg32_%d" % h for h in range(4)])
                    P.op("pool", lambda e: e.memset(Sgb[:], 0.0), writes=["Sgb"])
                    P.op("pool", lambda e: e.memset(Cm32[:], 0.0), writes=["Cm32_%d" % c for c in range(8)])
                    P.op("pool", lambda e: e.memset(Cmb[:], 0.0), writes=["Cmb"])
                b_S = palloc()
                for h in range(4):
                    P.op("pe", lambda e, h=h, b=b_S: e.matmul(psf[b][:, h * 128:(h + 1) * 128], lhsT=ks[:, h, :], rhs=qs[:, h, :],
                                                              start=True, stop=True),
                         reads=["ks", "qs"], writes=["ps%d" % b_S])
                P.op("dve", lambda e, b=b_S: e.tensor_tensor(out=sm[:], in0=psf[b][:, :].rearrange("p (h t) -> p h t", h=4),
                                                             in1=maskb[:].unsqueeze(1).to_broadcast([128, 4, 128]), op=ALU.mult),
                     reads=["ps%d" % b_S, "maskb"], writes=["vmT0"])
                b_gt = 9
                if g == 0:
                    b_gb = palloc()
                    P.op("pe", lambda e, b=b_gb: e.matmul(psf[b][:, 0:8], lhsT=ones_row[0:1, :], rhs=gbias_row, start=True, stop=True),
                         reads=["ones_row", "rowsb"], writes=["ps%d" % b_gb])
                    P.op("dve", lambda e, b=b_gb: e.tensor_copy(out=gbb[:], in_=psf[b][:, 0:8]), reads=["ps%d" % b_gb], writes=["gbb"])
                srcs = [(qmT, ["qmT0", "qmT1"]), (kmT, ["kmT0", "kmT1"]), (vmT, ["oT0a", "oT0b"])]
                for c in range(24):
                    tsrc, tk = srcs[c // 8]
                    P.op("pe", lambda e, c=c, tsrc=tsrc, b=b_gt: e.matmul(psf[b][:, 0:8], lhsT=tsrc[:, c % 8, :], rhs=wg[:, c, :],
                                                                          start=(c == 0), stop=False),
                         reads=tk + ["wg"], writes=[PK(b_gt)])
                P.op("pe", lambda e, b=b_gt: e.matmul(psf[b][:, 0:8], lhsT=ident[:], rhs=gbb[:], start=False, stop=True),
                     reads=["ident", "gbb"], writes=[PK(b_gt)])
                P.op("act", lambda e, b=b_gt: e.activation(out=gtmp[:], in_=psf[b][:, 4:8], func=AF.Exp, scale=-1.0),
                     reads=[PK(b_gt)], writes=["gtmp"])
                P.op("act", lambda e: e.activation(out=gl[:, 0:4], in_=gtmp[:], func=AF.Ln, bias=1.0), reads=["gtmp"], writes=["gl0"])
                P.op("act", lambda e, b=b_gt: e.activation(out=gl[:, 4:8], in_=psf[b][:, 0:4], func=AF.Copy),
                     reads=[PK(b_gt)], writes=["gl1"])
                b_o = []
                for half in range(2):
                    b = palloc()
                    b_o.append(b)
                    for hh in range(2):
                        h = half * 2 + hh
                        P.op("pe", lambda e, h=h, hh=hh, b=b: e.matmul(psf[b][:, hh * 256:(hh + 1) * 256], lhsT=sm[:, h, :],
                                                                       rhs=vg[:, h * 256:(h + 1) * 256], start=True, stop=False),
                             reads=["vmT0", "vg0", "vg1"], writes=["ps%d" % b])
                        P.op("pe", lambda e, h=h, hh=hh, b=b: e.matmul(psf[b][:, hh * 256:(hh + 1) * 256], lhsT=qs[:, h, :],
                                                                       rhs=Sgb[:, h, :], start=False, stop=True),
                             reads=["qs", "Sgb"], writes=["ps%d" % b])
                for h in range(4):
                    b = b_o[h // 2]
                    hh = h % 2
                    P.op("act", lambda e, h=h, hh=hh, b=b: e.activation(out=junkh[h], in_=psf[b][:, hh * 256:(hh + 1) * 256],
                                                                        func=AF.Square, accum_out=st_g[:, h:h + 1]),
                         reads=["ps%d" % b], writes=["st_g0_%d" % h, "oT1_%d" % h])
                SG0 = ["st_g0_%d" % h for h in range(4)]
                P.op("act", lambda e: e.activation(out=st_g[:, 4:8], in_=st_g[:, 0:4], func=AF.Ln, scale=1.0 / 256, bias=EPS),
                     reads=SG0, writes=["st_g1"])
                P.op("act", lambda e: e.activation(out=st_g[:, 8:12], in_=st_g[:, 4:8], func=AF.Exp, scale=-0.5),
                     reads=["st_g1"], writes=["st_g2"])
                for h in range(4):
                    b = b_o[h // 2]
                    hh = h % 2
                    P.op("dve", lambda e, h=h, hh=hh, b=b: e.scalar_tensor_tensor(
                        out=obf[:, h * 256:(h + 1) * 256], in0=psf[b][:, hh * 256:(hh + 1) * 256], scalar=st_g[:, 8 + h:9 + h],
                        in1=szg[:, h * 256:(h + 1) * 256], op0=ALU.mult, op1=ALU.mult),
                        reads=["ps%d" % b, "st_g2", "szg0", "szg1"], writes=["obf_g%d" % h])
                for half in range(2):
                    b = palloc()
                    for hh in range(2):
                        h = half * 2 + hh
                        P.op("pe", lambda e, h=h, hh=hh, b=b: e.matmul(psf[b][:, hh * 256:(hh + 1) * 256], lhsT=kdec[:, h * 128:(h + 1) * 128],
                                                                       rhs=vg[:, h * 256:(h + 1) * 256], start=True, stop=True),
                             reads=["kdec", "vg0", "vg1"], writes=["ps%d" % b])
                    for hh in range(2):
                        h = half * 2 + hh
                        P.op("dve", lambda e, h=h, hh=hh, b=b: e.scalar_tensor_tensor(
                            out=Sg32[:, h, :], in0=Sg32[:, h, :], scalar=scrA[:, h * 128 + 127:h * 128 + 128],
                            in1=psf[b][:, hh * 256:(hh + 1) * 256], op0=ALU.mult, op1=ALU.add),
                            reads=["Sg32_%d" % h, "A0", "A1", "ps%d" % b], writes=["Sg32_%d" % h])

                b_kt2 = []
                for half in range(2):
                    b = palloc(avoid=b_kt2, hold=True)
                    b_kt2.append(b)
                    for cc in range(4):
                        c = half * 4 + cc
                        P.op("pe", lambda e, c=c, cc=cc, b=b: e.matmul(psf[b][:, cc * 128:(cc + 1) * 128], lhsT=cactT[:, c, :],
                                                                       rhs=Wk[:, c, :], start=True, stop=True),
                             reads=CK + ["Wk"], writes=["ps%d" % b])
                b_Sm = palloc(hold=True)
                for h in range(4):
                    for dc in range(2):
                        P.op("pe", lambda e, h=h, dc=dc, b=b_Sm: e.matmul(psf[b][:, h * 128:(h + 1) * 128], lhsT=kmT[:, 2 * h + dc, :],
                                                                          rhs=qmT[:, 2 * h + dc, :], start=(dc == 0), stop=(dc == 1)),
                             reads=["kmT0", "kmT1", "qmT0", "qmT1"], writes=["ps%d" % b_Sm])
                b_ga = 10
                ga = psf[b_ga]
                GL = ["gl0", "gl1"]
                P.op("pe", lambda e, ga=ga: e.matmul(ga[:, 0:4], lhsT=triN[:], rhs=gl[:, 0:4], start=True, stop=True),
                     reads=GL + ["triN"], writes=[PK(b_ga)])
                P.op("pe", lambda e, ga=ga: e.matmul(ga[:, 4:8], lhsT=onesN[:], rhs=gl[:, 0:4], start=True, stop=True),
                     reads=GL + ["onesN"], writes=[PK(b_ga)])
                P.op("pe", lambda e, ga=ga: e.matmul(ga[:, 8:12], lhsT=maskf[:], rhs=gl[:, 0:4], start=True, stop=False),
                     reads=GL + ["maskf"], writes=[PK(b_ga)])
                P.op("pe", lambda e, ga=ga: e.matmul(ga[:, 8:12], lhsT=identf[:], rhs=gl[:, 4:8], start=False, stop=True),
                     reads=GL + ["identf"], writes=[PK(b_ga)])
                P.op("pe", lambda e, ga=ga: e.matmul(ga[:, 12:16], lhsT=UN[:], rhs=gl[:, 0:4], start=True, stop=False),
                     reads=GL + ["UN"], writes=[PK(b_ga)])
                P.op("pe", lambda e, ga=ga: e.matmul(ga[:, 12:16], lhsT=identf[:], rhs=gl[:, 4:8], start=False, stop=True),
                     reads=GL + ["identf"], writes=[PK(b_ga)])
                P.op("act", lambda e, ga=ga: e.activation(out=gex[:, 0:8], in_=ga[:, 0:8], func=AF.Exp), reads=[PK(b_ga)], writes=["gex0"])
                P.op("act", lambda e, ga=ga: e.activation(out=gex[:, 16:20], in_=ga[:, 0:4], func=AF.Exp, scale=-2.0),
                     reads=[PK(b_ga)], writes=["gex2"])
                P.op("act", lambda e, ga=ga: e.activation(out=gex[:, 8:16], in_=ga[:, 8:16], func=AF.Exp, bias=-LN16),
                     reads=[PK(b_ga)], writes=["gex1"])
                yield "MS"
                for half in range(2):
                    b = b_kt2[half]
                    for hh in range(2):
                        h = half * 2 + hh
                        P.op("dve", lambda e, h=h, hh=hh, b=b: e.tensor_scalar(
                            out=kw[:, h * 256:(h + 1) * 256], in0=psf[b][:, hh * 256:(hh + 1) * 256],
                            scalar1=gex[:, 12 + h:13 + h], scalar2=None, op0=ALU.mult),
                            reads=["ps%d" % b, "gex1"], writes=["kw%d" % h])
                prel(*b_kt2)
                for h in range(4):
                    P.op("dve", lambda e, h=h, b=b_Sm: e.scalar_tensor_tensor(
                        out=smm[:, h, :], in0=psf[b][:, h * 128:(h + 1) * 128], scalar=gex[:, 8 + h:9 + h], in1=maskb[:],
                        op0=ALU.mult, op1=ALU.mult),
                        reads=["ps%d" % b_Sm, "gex1", "maskb"], writes=["R%d" % h])
                prel(b_Sm)
                b_P2 = [palloc(hold=True)]
                b_P2.append(palloc(avoid=b_P2, hold=True))
                b_den = 11
                PH = lambda h: psf[b_P2[h // 2]][:, (h % 2) * 256:(h % 2 + 1) * 256]
                for h in range(4):
                    pk = "ps%d" % b_P2[h // 2]
                    P.op("pe", lambda e, h=h: e.matmul(PH(h), lhsT=smm[:, h, :], rhs=vaug[:, h, 0:256], start=True, stop=False),
                         reads=["R%d" % h, "vaug0", "vaug1"], writes=[pk])
                    for dc in range(2):
                        P.op("pe", lambda e, h=h, dc=dc: e.matmul(PH(h), lhsT=qmT[:, 2 * h + dc, :], rhs=Cmb[:, 2 * h + dc, 0:256],
                                                                  start=False, stop=(dc == 1)),
                             reads=["qmT0", "qmT1", "Cmb"], writes=[pk])
                for h in range(4):
                    dk = PK(b_den)
                    P.op("pe", lambda e, h=h: e.matmul(psf[b_den][:, 2 * h:2 * h + 2], lhsT=smm[:, h, :], rhs=vaug[:, h, 255:257], start=True, stop=False),
                         reads=["R%d" % h, "vaug0", "vaug1"], writes=[dk])
                    for dc in range(2):
                        P.op("pe", lambda e, h=h, dc=dc: e.matmul(psf[b_den][:, 2 * h:2 * h + 2], lhsT=qmT[:, 2 * h + dc, :], rhs=Cmb[:, 2 * h + dc, 255:257],
                                                                  start=False, stop=(dc == 1)),
                             reads=["qmT0", "qmT1", "Cmb"], writes=[dk])
                b_P = b_P2 + [b_den]
                for h in range(4):
                    for dc in range(2):
                        b = palloc(avoid=b_P)
                        P.op("pe", lambda e, h=h, dc=dc, b=b: e.matmul(psf[b][:, 0:257], lhsT=kw[:, h * 256 + dc * 128:h * 256 + (dc + 1) * 128],
                                                                       rhs=vaug[:, h, :], start=True, stop=True),
                             reads=["kw%d" % h, "vaug0", "vaug1"], writes=["ps%d" % b])
                        P.op("dve", lambda e, h=h, dc=dc, b=b: e.scalar_tensor_tensor(
                            out=Cm32[:, 2 * h + dc, :], in0=Cm32[:, 2 * h + dc, :], scalar=gex[:, 4 + h:5 + h],
                            in1=psf[b][:, 0:257], op0=ALU.mult, op1=ALU.add),
                            reads=["Cm32_%d" % (2 * h + dc), "gex0", "ps%d" % b], writes=["Cm32_%d" % (2 * h + dc)])

                yield "MP"
                for h in range(4):
                    P.op("act", lambda e, h=h: e.activation(out=junkh[h], in_=PH(h), func=AF.Square, accum_out=st_m[:, h:h + 1]),
                         reads=["ps%d" % b_P2[h // 2]], writes=["st_m_ssq%d" % h, "oT1_%d" % h])
                P.op("act", lambda e: e.activation(out=st_m[:, 4:8], in_=psf[b_den][:, 0:8].rearrange("p (h two) -> p h two", two=2)[:, :, 1], func=AF.Square),
                     reads=[PK(b_den)], writes=["st_m_den"])
                SSQ = ["st_m_ssq%d" % h for h in range(4)]
                P.op("dve", lambda e: e.tensor_tensor(out=st_m[:, 8:12], in0=st_m[:, 4:8], in1=gex[:, 16:20], op=ALU.max),
                     reads=["st_m_den", "gex2"], writes=["st_m_D"])
                P.op("dve", lambda e: e.scalar_tensor_tensor(out=st_m[:, 24:28], in0=st_m[:, 8:12], scalar=EPS * 256.0, in1=st_m[:, 0:4],
                                                             op0=ALU.mult, op1=ALU.add),
                     reads=["st_m_D"] + SSQ, writes=["st_m_v"])
                P.op("act", lambda e: e.activation(out=st_m[:, 28:32], in_=st_m[:, 24:28], func=AF.Ln, scale=1.0 / 256),
                     reads=["st_m_v"], writes=["st_m_ln"])
                P.op("act", lambda e: e.activation(out=st_m[:, 36:40], in_=st_m[:, 28:32], func=AF.Exp, scale=-0.5),
                     reads=["st_m_ln"], writes=["st_m_s"])
                t1 = scrA
                for h in range(4):
                    P.op("dve", lambda e, h=h: e.scalar_tensor_tensor(
                        out=t1[:, h * 256:(h + 1) * 256], in0=PH(h), scalar=st_m[:, 36 + h:37 + h],
                        in1=gmn[:, h * 256:(h + 1) * 256], op0=ALU.mult, op1=ALU.mult),
                        reads=["ps%d" % b_P2[h // 2], "st_m_s", "gmn"], writes=["A%d" % h])
                prel(*b_P2)
                for half in range(2):
                    b = palloc()
                    for cc in range(4):
                        c = half * 4 + cc
                        P.op("pe", lambda e, c=c, cc=cc, b=b: e.matmul(psf[b][:, cc * 128:(cc + 1) * 128], lhsT=cactT[:, c, :],
                                                                       rhs=dskip[:, c, :], start=True, stop=True),
                             reads=CK + ["dskip"], writes=["ps%d" % b])
                    P.op("dve", lambda e, half=half, b=b: e.tensor_tensor(out=t1[:, half * 512:(half + 1) * 512], in0=psf[b][:, :],
                                                                          in1=t1[:, half * 512:(half + 1) * 512], op=ALU.add),
                         reads=["ps%d" % b, "A%d" % (2 * half), "A%d" % (2 * half + 1)], writes=["A%d" % (2 * half), "A%d" % (2 * half + 1)])
                    P.op("dve", lambda e, half=half: e.tensor_tensor(out=obf[:, D + half * 512:D + (half + 1) * 512],
                                                                     in0=t1[:, half * 512:(half + 1) * 512],
                                                                     in1=szm[:, half * 512:(half + 1) * 512], op=ALU.mult),
                         reads=["A%d" % (2 * half), "A%d" % (2 * half + 1), "szm%d" % half], writes=["obf_m%d" % half])
                P.op("act", lambda e: e.activation(out=Sgb[:].rearrange("p h e -> p (h e)"), in_=Sg32[:].rearrange("p h e -> p (h e)"), func=AF.Copy),
                     reads=["Sg32_%d" % h for h in range(4)], writes=["Sgb"])
                P.op("act", lambda e: e.activation(out=dmy[:, 1:2], in_=dmy[:, 0:1], func=AF.Silu), reads=["dmy0"], writes=["dmy1"])
                yield "PP"
                if g == 0:
                    for c in range(8):
                        P.op("dve", lambda e, c=c: e.tensor_scalar(out=wout[:, c, :], in0=wout[:, c, :], scalar1=p128[:, 8 + c:9 + c],
                                                                    scalar2=None, op0=ALU.mult),
                             reads=["wout0", "p128"], writes=["wout0"])
                OB = ["obf_g%d" % h for h in range(4)] + ["obf_m0", "obf_m1"]
                for half in range(2):
                    b = palloc()
                    for cc in range(8):
                        c = half * 8 + cc
                        P.op("pe", lambda e, c=c, cc=cc, b=b: e.transpose(out=psb[b][:, cc * 128:(cc + 1) * 128],
                                                                          in_=obf[:, c * 128:(c + 1) * 128], identity=ident[:]),
                             reads=(OB[0:4] if half == 0 else OB[4:6]) + ["ident"], writes=["ps%d" % b])
                    if half == 0:
                        P.op("act", lambda e, b=b: e.activation(out=oT[:, 0:8, :], in_=psb[b][:, 0:1024].rearrange("p (c t) -> p c t", c=8),
                                                                func=AF.Copy),
                             reads=["ps%d" % b], writes=["oT0a", "oT0b"])
                    else:
                        P.op("dve", lambda e, b=b: e.tensor_copy(out=oT[:, 8:16, :], in_=psb[b][:, 0:1024].rearrange("p (c t) -> p c t", c=8)),
                             reads=["ps%d" % b], writes=OTK[1:])
                P.op("dve", lambda e: e.tensor_copy(out=Cmb[:].rearrange("p h e -> p (h e)"), in_=Cm32[:].rearrange("p h e -> p (h e)")),
                     reads=["Cm32_%d" % c for c in range(8)], writes=["Cmb"])
                P.op("act", lambda e: e.activation(out=dmy[:, 3:4], in_=dmy[:, 2:3], func=AF.Exp), reads=["dmy2"], writes=["dmy3"])
                yield "FT"
                P.op("sp", lambda e, g=g: e.dma_start(out=yt[:], in_=x_d[g * 128:(g + 1) * 128, :]), writes=OB, chan="xr")
                b_y = [palloc()]
                b_y.append(palloc(avoid=b_y))
                for kk in range(2):
                    for half in range(2):
                        b = b_y[half]
                        for c in range(kk * 8, kk * 8 + 8):
                            P.op("pe", lambda e, c=c, half=half, b=b: e.matmul(psf[b][:, :], lhsT=oT[:, c, :],
                                                                               rhs=wout[:, c, half * 512:(half + 1) * 512],
                                                                               start=(c == 0), stop=(c == 15)),
                                 reads=((["oT0a"] if c < 4 else ["oT0b"]) + ["wout0"] if c < 8 else OTK[1:] + ["wout1"]), writes=["ps%d" % b])
                for half in range(2):
                    b = b_y[half]
                    P.op("dve", lambda e, half=half, b=b: e.tensor_tensor(out=yt[:, half * 512:(half + 1) * 512], in0=psf[b][:, :],
                                                                          in1=yt[:, half * 512:(half + 1) * 512], op=ALU.add),
                         reads=["ps%d" % b] + (OB[0:4] if half == 0 else OB[4:6]), writes=(OB[0:4] if half == 0 else OB[4:6]))
                yield "F"
                P.op("act", lambda e: e.activation(out=junk, in_=yt[:], func=AF.Square, accum_out=st_y[:, 0:1]),
                     reads=OB, writes=["st_y0", "oT0a", "oT0b"])
                P.op("act", lambda e: e.activation(out=st_y[:, 1:2], in_=st_y[:, 0:1], func=AF.Ln, scale=1.0 / D, bias=EPS),
                     reads=["st_y0"], writes=["st_y1"])
                P.op("act", lambda e: e.activation(out=st_y[:, 2:3], in_=st_y[:, 1:2], func=AF.Exp, scale=-0.5),
                     reads=["st_y1"], writes=["st_y2"])
                P.op("dve", lambda e: e.scalar_tensor_tensor(out=yt[:], in0=yt[:], scalar=st_y[:, 2:3], in1=gfin[:], op0=ALU.mult, op1=ALU.mult),
                     reads=OB + ["st_y2", "gfin"], writes=OB)
                ok = "out%d" % g
                out_keys.append(ok)
                P.op("sp", lambda e, g=g: e.dma_start(out=out_d[g * 128:(g + 1) * 128, :], in_=yt[:]),
                     reads=OB, writes=[ok], chan="st")

        tiles = [(s * ntile + ti, s, ti) for s in range(nseq) for ti in range(ntile)]
        gens = [tile(*t) for t in tiles]

        def run(gi, upto):
            if gi >= len(gens):
                return
            for tag in gens[gi]:
                if tag == upto:
                    return
            assert upto is None, (gi, upto)

        run(0, "A1"); run(0, "A"); run(0, "B1"); run(0, "CV"); run(0, "Z"); run(0, "QP"); run(1, "A1")
        for n in range(len(gens)):
            run(n, "TM")
            run(n + 1, "A")
            run(n, "MS")
            run(n + 1, "B1")
            run(n, "MP")
            run(n, "PP")
            run(n + 1, "CV")
            run(n + 1, "Z")
            run(n, "FT")
            run(n + 1, "QP")
            run(n + 2, "A1")
            run(n, None)

        P.op("sp", lambda e: e.nop(), reads=out_keys)
        P.emit()
    return nc


def prep_shared(norm_g, w_in, w_gla_gate_up, b_gla_gate, gla_norm_g, conv_w, conv_b,
                w_q_m, w_k_m, w_v_m, w_igate, b_igate, w_fgate, b_fgate,
                mlstm_norm_g, mlstm_skip, w_out, final_norm_g):
    f = lambda a: np.ascontiguousarray(np.asarray(a, dtype=np.float32))
    pp = lambda v: f(v).reshape(8, 128).T
    p128 = np.concatenate([pp(norm_g), pp(f(gla_norm_g).reshape(-1)), pp(mlstm_skip)]
                          + [pp(f(conv_w)[k]) for k in range(4)] + [pp(conv_b)], axis=1)
    rows = np.concatenate([f(conv_b).reshape(1, -1), f(b_igate).reshape(1, -1), f(b_fgate).reshape(1, -1)], axis=1)
    wup = np.concatenate([f(w_gla_gate_up), f(b_gla_gate).reshape(1, -1), np.zeros((111, 512), np.float32)], axis=0)

    def expand(w):
        w = f(w)
        o = np.zeros((1024, 128), np.float32)
        for n in range(256):
            c, nl = divmod(n, 32)
            o[c * 128 + 4 * nl:c * 128 + 4 * nl + 4, 4 * nl:4 * nl + 4] = w[n]
        return o

    wg = np.concatenate([f(w_igate), f(w_fgate)], axis=1)
    wg = wg.reshape(24, 128, 8).transpose(1, 0, 2).reshape(128, 192)
    return {
        "w_in": f(w_in), "w_out": f(w_out), "p128": f(p128), "rows": f(rows), "wup": f(wup),
        "wq": expand(w_q_m), "wk": expand(w_k_m), "wv": expand(w_v_m), "wg": f(wg),
        "gmn": f(mlstm_norm_g).reshape(1, -1), "gfin": f(final_norm_g).reshape(1, -1),
    }


_NC_CACHE = {}


def kernel(x, **params):
    x = np.asarray(x, dtype=np.float32)
    bsz, seq, _ = x.shape
    nseq = bsz // NCORES
    ntile = seq // 128
    key = (nseq, ntile)
    if key not in _NC_CACHE:
        _NC_CACHE[key] = build(nseq, ntile)
    nc = _NC_CACHE[key]
    shared = prep_shared(**params)
    in_maps = []
    for c in range(NCORES):
        m = dict(shared)
        m["x"] = np.ascontiguousarray(x[c * nseq:(c + 1) * nseq].reshape(nseq * seq, D))
        in_maps.append(m)
    res = run_bass_kernel_spmd(nc, in_maps, core_ids=list(range(NCORES)))
    outs = [np.asarray(r["out"]).reshape(nseq, seq, D) for r in res.results]
    return np.concatenate(outs, axis=0).astype(np.float32)
```

```python
import contextlib
import math
import numpy as np
import concourse.bass as bass
import concourse.mybir as mybir
from concourse.bass_utils import run_bass_kernel_spmd

F32 = mybir.dt.float32
BF16 = mybir.dt.bfloat16
AF = mybir.ActivationFunctionType
ALU = mybir.AluOpType

D = 1024
PW = 5136
NCORES = 8
ENGS = ("pe", "act", "dve", "pool", "sp")
EPS = 1e-6
LN16 = math.log(16.0)


class Prog:
    def __init__(self, nc):
        self.nc = nc
        self.ops = []
        self.last_use = {}

    def op(self, eng, fn, reads=(), writes=(), chan=None):
        for k in tuple(reads) + tuple(writes):
            if k.startswith("ps"):
                self.last_use[k] = len(self.ops)
        self.ops.append((eng, fn, tuple(reads), tuple(writes), chan))

    def emit(self):
        nc, ops = self.nc, self.ops
        n = len(ops)
        deps = [set() for _ in range(n)]
        last_w, readers = {}, {}
        needs_inc = [False] * n
        for i, (eng, fn, reads, writes, chan) in enumerate(ops):
            for k in reads:
                if k in last_w:
                    deps[i].add(last_w[k])
            for k in writes:
                if k in last_w:
                    deps[i].add(last_w[k])
                for r in readers.get(k, ()):
                    if r != i:
                        deps[i].add(r)
            for k in reads:
                readers.setdefault(k, []).append(i)
            for k in writes:
                last_w[k] = i
                readers[k] = []

        def semkey(i):
            eng, _, _, _, chan = ops[i]
            return ("D", chan) if chan is not None else ("E", eng)

        for i in range(n):
            eng = ops[i][0]
            rm = set()
            for j in deps[i]:
                if ops[j][4] is None and ops[i][4] is None and ops[j][0] == "pe" and eng == "pe":
                    rm.add(j)
            deps[i] -= rm
            last = {}
            for j in deps[i]:
                sk = semkey(j)
                if sk not in last or j > last[sk]:
                    last[sk] = j
            deps[i] = set(last.values())
            for j in deps[i]:
                needs_inc[j] = True
        for i in range(n):
            if ops[i][4] is not None:
                needs_inc[i] = True
        counts, ticket = {}, [None] * n
        for i in range(n):
            if needs_inc[i]:
                sk = semkey(i)
                counts[sk] = counts.get(sk, 0) + (16 if sk[0] == "D" else 1)
                ticket[i] = (sk, counts[sk])
        with contextlib.ExitStack() as st:
            sems = {sk: st.enter_context(nc.semaphore("s_%s_%s" % sk)) for sk in sorted(counts)}
            block = st.enter_context(nc.Block())
            handles = {"pe": block.tensor, "act": block.scalar, "dve": block.vector,
                       "pool": block.gpsimd, "sp": block.sync}
            for eng in ENGS:
                my = [i for i in range(n) if ops[i][0] == eng]
                if not my:
                    continue

                def body(e, my=my):
                    waited = {}
                    for i in my:
                        for j in sorted(deps[i]):
                            sk, v = ticket[j]
                            if waited.get(sk, 0) < v:
                                e.wait_ge(sems[sk], v)
                                waited[sk] = v
                        ins = ops[i][1](e)
                        if needs_inc[i]:
                            sk, v = ticket[i]
                            ins.then_inc(sems[sk], 16 if sk[0] == "D" else 1)

                handles[eng](body)


def build(nseq=2, ntile=16):
    ntok = nseq * ntile * 128
    nc = bass.Bass("TRN2", target_bir_lowering=False)
    dram = lambda name, shape, kind="ExternalInput": nc.dram_tensor(name, shape, F32, kind=kind).ap()
    x_d = dram("x", [ntok, D])
    win_d = dram("w_in", [D, PW])
    wout_d = dram("w_out", [2 * D, D])
    p128_d = dram("p128", [128, 64])
    rows_d = dram("rows", [1, 1032])
    wup_d = dram("wup", [128, 512])
    wq_d = dram("wq", [D, 128])
    wk_d = dram("wk", [D, 128])
    wv_d = dram("wv", [D, 128])
    wg_d = dram("wg", [128, 192])
    gmn_d = dram("gmn", [1, D])
    gfin_d = dram("gfin", [1, D])
    out_d = dram("out", [ntok, D], kind="ExternalOutput")

    st = contextlib.ExitStack()
    with st:
        sb = lambda name, shape, dt: st.enter_context(nc.sbuf_tensor(name, shape, dt))
        P = Prog(nc)
        win = sb("win", [128, 8, PW], BF16)
        wout = sb("wout", [128, 16, D], BF16)
        Wq = sb("Wq", [128, 8, 128], BF16)
        Wk = sb("Wk", [128, 8, 128], BF16)
        Wv = sb("Wv", [128, 8, 128], BF16)
        dconv = sb("dconv", [128, 4, 8, 128], BF16)
        dskip = sb("dskip", [128, 8, 128], BF16)
        wg = sb("wgt", [128, 24, 8], BF16)
        gmn = sb("gmn_bc", [128, D], F32)
        gfin = sb("gfin_bc", [128, D], F32)
        p128 = sb("p128t", [128, 64], F32)
        rowsb = sb("rowsb", [1, 1032], BF16)
        wup = sb("wupt", [128, 512], BF16)
        identf = sb("identf", [128, 128], F32)
        ident = sb("ident", [128, 128], BF16)
        maskf = sb("maskf", [128, 128], F32)
        maskb = sb("maskb", [128, 128], BF16)
        triG = sb("triG", [128, 128], BF16)
        UG = sb("UG", [128, 128], BF16)
        triN = sb("triN", [128, 128], F32)
        UN = sb("UN", [128, 128], F32)
        onesN = sb("onesN", [128, 128], F32)
        ones_row = sb("ones_row", [1, 128], BF16)
        raug = sb("raug", [128, 128], BF16)
        gbb = sb("gbb", [128, 8], BF16)
        Sg32 = sb("Sg32", [128, 4, 256], F32)
        Sgb = sb("Sgb", [128, 4, 256], BF16)
        Cm32 = sb("Cm32", [128, 8, 257], F32)
        Cmb = sb("Cmb", [128, 8, 257], BF16)
        xt = sb("xt", [128, D], F32)
        xs = sb("xs", [128, D], BF16)
        xT = sb("xT", [128, 8, 128], BF16)
        xme = sb("xme", [128, 8, 131], BF16)
        vg = sb("vg", [128, D], BF16)
        szg = sb("szg", [128, D], BF16)
        szm = sb("szm", [128, D], BF16)
        scrA = sb("scrA", [128, 1024], F32)
        scrB = sb("scrB", [128, 512], F32)
        qs = sb("qs", [128, 4, 128], BF16)
        ks = sb("ks", [128, 4, 128], BF16)
        kdec = sb("kdec", [128, 512], BF16)
        spb = kdec
        cactT = sb("cactT", [128, 8, 128], BF16)
        qmT = sb("qmT", [128, 8, 128], BF16)
        kmT = sb("kmT", [128, 8, 128], BF16)
        smb = sb("smb", [128, 8, 128], BF16)
        sm = smb[:, 0:4, :]
        smm = smb[:, 4:8, :]
        vaug = sb("vaug", [128, 4, 257], BF16)
        kw = sb("kw", [128, D], BF16)
        obf = sb("obf", [128, 2 * D], BF16)
        oT = sb("oT", [128, 16, 128], BF16)
        yt = obf[:, :].bitcast(F32)
        junk = oT[:, 0:8, :].rearrange("p c t -> p (c t)")
        vmT = oT[:, 0:8, :]
        junkh = [oT[:, 8 + 2 * h:10 + 2 * h, :].rearrange("p c t -> p (c t)") for h in range(4)]
        OTK = ["oT0", "oT1_0", "oT1_1", "oT1_2", "oT1_3"]
        st_x = sb("st_x", [128, 4], F32)
        st_g = sb("st_g", [128, 12], F32)
        gl = sb("gl", [128, 8], F32)
        gtmp = sb("gtmp", [128, 4], F32)
        gex = sb("gex", [128, 20], F32)
        st_m = sb("st_m", [128, 40], F32)
        st_y = sb("st_y", [128, 4], F32)
        dmy = sb("dmy", [128, 4], F32)

        psf, psb = [], []
        for b in range(8):
            t = st.enter_context(nc.psum_tensor("ps%d" % b, [128, 512], F32))
            psf.append(t)
            psb.append(t.bitcast(BF16))
        psf += [psf[7][:, 0:128], psf[7][:, 128:144], psf[7][:, 144:160], psf[7][:, 160:168]]
        pctr = [0]
        PK = lambda b: "ps%d" % min(b, 7)

        held = set()

        def palloc(avoid=(), hold=False):
            cands = [b for b in range(7) if b not in avoid and b not in held]
            assert cands, "out of PSUM banks"
            b = min(cands, key=lambda b: P.last_use.get("ps%d" % b, -1 - (8 - b)))
            P.last_use["ps%d" % b] = len(P.ops)
            if hold:
                held.add(b)
            return b

        def prel(*banks):
            for b in banks:
                held.discard(b)

        P.op("pool", lambda e: e.memset(identf[:], 0.0), writes=["identf"])
        P.op("pool", lambda e: e.affine_select(out=identf[:], in_=identf[:], pattern=[[-1, 128]],
                                               compare_op=ALU.not_equal, fill=1.0, base=0, channel_multiplier=1),
             reads=["identf"], writes=["identf"])
        P.op("pool", lambda e: e.memset(maskf[:], 1.0), writes=["maskf"])
        P.op("pool", lambda e: e.affine_select(out=maskf[:], in_=maskf[:], pattern=[[1, 128]],
                                               compare_op=ALU.is_ge, fill=0.0, base=0, channel_multiplier=-1),
             reads=["maskf"], writes=["maskf"])
        WGRP = [(3072, 4112), (2048, 3072), (4112, 5136), (0, 1024), (1024, 2048)]

        def load_wgroup(gi):
            c0, c1 = WGRP[gi]
            for k in range(8):
                P.op("pool", lambda e, k=k, c0=c0, c1=c1: e.dma_start(out=win[:, k, c0:c1], in_=win_d[k * 128:(k + 1) * 128, c0:c1],
                                                                      max_dma_last_dim=4096),
                     writes=["win%d_%d" % (gi, k)], chan="win%d_%d" % (gi, k))

        def wkey(col0, k):
            for gi, (c0, c1) in enumerate(WGRP):
                if c0 <= col0 < c1:
                    return "win%d_%d" % (gi, k)
            raise KeyError(col0)
        P.op("sp", lambda e: e.dma_start(out=p128[:], in_=p128_d), writes=["p128"], chan="p128")
        P.op("pool", lambda e: e.dma_start(out=wup[:], in_=wup_d), writes=["wup"], chan="wup")
        load_wgroup(0)
        P.op("pool", lambda e: e.dma_start(out=rowsb[:], in_=rows_d), writes=["rowsb"], chan="rows")
        P.op("sp", lambda e: e.dma_start(out=gmn[:], in_=gmn_d.partition_broadcast(128)), writes=["gmn"], chan="gmn")
        P.op("sp", lambda e: e.dma_start(out=gfin[:], in_=gfin_d.partition_broadcast(128)), writes=["gfin"], chan="gfin")
        load_wgroup(1)
        load_wgroup(2)
        load_wgroup(3)
        P.op("pool", lambda e: e.dma_start(out=Wq[:], in_=wq_d.rearrange("(c p) n -> p c n", p=128)), writes=["Wq"], chan="Wq")
        P.op("pool", lambda e: e.dma_start(out=Wk[:], in_=wk_d.rearrange("(c p) n -> p c n", p=128)), writes=["Wk"], chan="Wk")
        P.op("pool", lambda e: e.dma_start(out=Wv[:], in_=wv_d.rearrange("(c p) n -> p c n", p=128)), writes=["Wv"], chan="Wv")
        P.op("pool", lambda e: e.dma_start(out=wg[:], in_=wg_d.rearrange("p (c j) -> p c j", j=8)), writes=["wg"], chan="wg")
        load_wgroup(4)
        for kk in range(2):
            P.op("pool", lambda e, kk=kk: e.dma_start(
                out=wout[:, kk * 8:(kk + 1) * 8, :],
                in_=wout_d[kk * 1024:(kk + 1) * 1024, :].rearrange("(c p) n -> p c n", p=128)),
                writes=["wout%d" % kk], chan="wout%d" % kk)

        P.op("dve", lambda e: e.tensor_copy(out=ident[:], in_=identf[:]), reads=["identf"], writes=["ident"])
        P.op("dve", lambda e: e.tensor_copy(out=maskb[:], in_=maskf[:]), reads=["maskf"], writes=["maskb"])
        P.op("dve", lambda e: e.tensor_scalar(out=triG[:], in0=maskf[:], scalar1=-1.0 / 16, scalar2=None, op0=ALU.mult),
             reads=["maskf"], writes=["triG"])
        P.op("dve", lambda e: e.tensor_scalar(out=UG[:], in0=maskf[:], scalar1=-1.0, scalar2=1.0 / 16, op0=ALU.add, op1=ALU.mult),
             reads=["maskf"], writes=["UG"])
        P.op("dve", lambda e: e.tensor_scalar(out=triN[:], in0=maskf[:], scalar1=-1.0, scalar2=None, op0=ALU.mult),
             reads=["maskf"], writes=["triN"])
        P.op("dve", lambda e: e.tensor_scalar(out=UN[:], in0=maskf[:], scalar1=-1.0, scalar2=None, op0=ALU.add),
             reads=["maskf"], writes=["UN"])
        P.op("dve", lambda e: e.memset(onesN[:], -1.0), writes=["onesN"])
        P.op("dve", lambda e: e.memset(dmy[:], 0.0), writes=["dmy0", "dmy1", "dmy2", "dmy3"])
        P.op("dve", lambda e: e.memset(ones_row[:], 1.0), writes=["ones_row"])
        P.op("dve", lambda e: e.memset(raug[:], 1.0), writes=["raug"])
        P.op("dve", lambda e: e.memset(vaug[:], 1.0), writes=["vaug"])
        for k in range(4):
            for c in range(8):
                P.op("dve", lambda e, k=k, c=c: e.tensor_scalar(out=dconv[:, k, c, :], in0=identf[:],
                                                                 scalar1=p128[:, 24 + k * 8 + c:25 + k * 8 + c],
                                                                 scalar2=None, op0=ALU.mult),
                     reads=["identf", "p128"], writes=["dconv"])
        for c in range(8):
            P.op("dve", lambda e, c=c: e.tensor_scalar(out=dskip[:, c, :], in0=identf[:], scalar1=p128[:, 16 + c:17 + c],
                                                        scalar2=None, op0=ALU.mult),
                 reads=["identf", "p128"], writes=["dskip"])

        gT = p128[:, 0:8]
        convb_row = rowsb[0:1, 0:1024]
        gbias_row = rowsb[0:1, 1024:1032]

        def load_x(g):
            P.op("sp", lambda e, g=g: e.dma_start(out=xt[:], in_=x_d[g * 128:(g + 1) * 128, :]),
                 writes=["xt"], chan="xt")

        load_x(0)
        out_keys = []
        def tile(g, s, ti):
            if True:
                first = (ti == 0)
                P.op("act", lambda e: e.activation(out=xs[:], in_=xt[:], func=AF.Square, accum_out=st_x[:, 0:1]),
                     reads=["xt"], writes=["st_x0", "xs"])
                P.op("act", lambda e: e.activation(out=st_x[:, 1:2], in_=st_x[:, 0:1], func=AF.Ln, scale=1.0 / D, bias=EPS),
                     reads=["st_x0"], writes=["st_x1"])
                P.op("act", lambda e: e.activation(out=st_x[:, 2:3], in_=st_x[:, 1:2], func=AF.Exp, scale=-0.5),
                     reads=["st_x1"], writes=["st_x2"])
                P.op("dve", lambda e: e.tensor_scalar(out=xs[:], in0=xt[:], scalar1=st_x[:, 2:3], scalar2=None, op0=ALU.mult),
                     reads=["xt", "st_x2"], writes=["xs"])
                if g + 1 < nseq * ntile:
                    load_x(g + 1)
                yield "A1"
                bT_ = palloc()
                for c in range(8):
                    P.op("pe", lambda e, c=c, b=bT_: e.transpose(out=psb[b][:, c * 128:(c + 1) * 128],
                                                                  in_=xs[:, c * 128:(c + 1) * 128], identity=ident[:]),
                         reads=["xs", "ident"], writes=["ps%d" % bT_])
                P.op("dve", lambda e, b=bT_: e.tensor_tensor(
                    out=xT[:], in0=psb[b][:, 0:1024].rearrange("p (c t) -> p c t", c=8),
                    in1=gT.unsqueeze(2).to_broadcast([128, 8, 128]), op=ALU.mult),
                    reads=["ps%d" % bT_, "p128"], writes=["xT"])

                yield "A"
                def proj_fm(col0, nch, M=128, bank=None, hold=False):
                    b = palloc(hold=hold) if bank is None else bank
                    for ch in range(nch):
                        for k in range(8):
                            P.op("pe", lambda e, ch=ch, k=k, b=b: e.matmul(
                                psf[b][0:M, ch * 128:(ch + 1) * 128],
                                lhsT=win[:, k, col0 + ch * M: col0 + (ch + 1) * M], rhs=xT[:, k, :],
                                start=(k == 0), stop=(k == 7)),
                                reads=["xT", wkey(col0, k)], writes=[PK(b)])
                    return b

                b_r = proj_fm(3072, 1, M=128, bank=8)
                P.op("dve", lambda e, b=b_r: e.tensor_copy(out=raug[0:16, :], in_=psf[b][0:16, 0:128]),
                     reads=[PK(b_r)], writes=["raug"])
                if first:
                    P.op("pool", lambda e: e.memset(xme[:, :, 0:3], 0.0), writes=["xme_h"])
                else:
                    P.op("pool", lambda e: e.tensor_copy(out=xme[:, :, 0:3], in_=xme[:, :, 128:131]),
                         reads=["xme0", "xme1"], writes=["xme_h"])
                b_x0 = proj_fm(3088, 4)
                b_x1 = proj_fm(3088 + 512, 4)
                P.op("act", lambda e, b=b_x0: e.activation(out=xme[:, 0:4, 3:131], in_=psf[b][:, :].rearrange("p (c t) -> p c t", c=4),
                                                           func=AF.Copy),
                     reads=["ps%d" % b_x0, "xme_h"], writes=["xme0"])
                P.op("act", lambda e, b=b_x1: e.activation(out=xme[:, 4:8, 3:131], in_=psf[b][:, :].rearrange("p (c t) -> p c t", c=4),
                                                           func=AF.Copy),
                     reads=["ps%d" % b_x1, "xme_h"], writes=["xme1"])
                b_pre = palloc()
                P.op("pe", lambda e, b=b_pre: e.matmul(psf[b][:, :], lhsT=raug[:], rhs=wup[:], start=True, stop=True),
                     reads=["raug", "wup"], writes=["ps%d" % b_pre])
                P.op("act", lambda e, b=b_pre: e.activation(out=scrB[:], in_=psf[b][:, :], func=AF.Exp, scale=-1.0),
                     reads=["ps%d" % b_pre], writes=["scrB"])
                P.op("act", lambda e: e.activation(out=spb[:], in_=scrB[:], func=AF.Ln, bias=1.0),
                     reads=["scrB"], writes=["kdec"])
                yield "B1"
                b_cv = []
                for half in range(2):
                    b = palloc()
                    b_cv.append(b)
                    for cc in range(4):
                        c = half * 4 + cc
                        for k in range(4):
                            P.op("pe", lambda e, c=c, cc=cc, k=k, b=b: e.matmul(
                                psf[b][:, cc * 128:(cc + 1) * 128], lhsT=dconv[:, k, c, :], rhs=xme[:, c, k:k + 128],
                                start=(k == 0), stop=(k == 3)),
                                reads=["xme%d" % (c // 4), "xme_h", "dconv"], writes=["ps%d" % b])
                for c in range(8):
                    P.op("act", lambda e, c=c, b=b_cv[c // 4]: e.activation(
                        out=cactT[:, c, :], in_=psf[b][:, (c % 4) * 128:(c % 4 + 1) * 128], func=AF.Silu, bias=p128[:, 56 + c:57 + c]),
                        reads=["ps%d" % b_cv[c // 4], "p128"], writes=["cactT%d" % c])
                yield "CV"
                def proj_tm(col0):
                    b = palloc()
                    for k in range(8):
                        P.op("pe", lambda e, k=k, b=b: e.matmul(psf[b][:, :], lhsT=xT[:, k, :], rhs=win[:, k, col0:col0 + 512],
                                                                start=(k == 0), stop=(k == 7)),
                             reads=["xT", wkey(col0, k)], writes=["ps%d" % b])
                    return b

                for half in range(2):
                    b = proj_tm(2048 + half * 512)
                    P.op("act", lambda e, b=b, half=half: e.activation(out=szg[:, half * 512:(half + 1) * 512], in_=psf[b][:, :], func=AF.Silu),
                         reads=["ps%d" % b], writes=["szg%d" % half])
                for half in range(2):
                    b = proj_tm(4112 + half * 512)
                    P.op("act", lambda e, b=b, half=half: e.activation(out=szm[:, half * 512:(half + 1) * 512], in_=psf[b][:, :], func=AF.Silu),
                         reads=["ps%d" % b], writes=["szm%d" % half])
                yield "Z"
                b_q = proj_fm(0, 4, hold=True)
                b_k = proj_fm(512, 4, hold=True)
                yield "QP"
                b_bT = palloc()
                for h in range(4):
                    P.op("pe", lambda e, h=h, b=b_bT: e.matmul(psf[b][:, h * 128:(h + 1) * 128], lhsT=spb[:, h * 128:(h + 1) * 128],
                                                               rhs=triG[:], start=True, stop=True),
                         reads=["kdec", "triG"], writes=["ps%d" % b_bT])
                b_bL = palloc()
                P.op("pe", lambda e, b=b_bL: e.matmul(psf[b][:, :], lhsT=UG[:], rhs=spb[:], start=True, stop=True),
                     reads=["kdec", "UG"], writes=["ps%d" % b_bL])
                Eq = scrA[:, 0:512]
                Ek = scrA[:, 512:1024]
                P.op("act", lambda e, b=b_bT: e.activation(out=Eq, in_=psf[b][:, :], func=AF.Exp), reads=["ps%d" % b_bT], writes=["A0", "A1"])
                P.op("act", lambda e, b=b_bT: e.activation(out=Ek, in_=psf[b][:, :], func=AF.Exp, scale=-1.0),
                     reads=["ps%d" % b_bT], writes=["A2", "A3"])
                P.op("act", lambda e, b=b_bL: e.activation(out=scrB[:], in_=psf[b][:, :], func=AF.Exp),
                     reads=["ps%d" % b_bL], writes=["scrB"])
                P.op("dve", lambda e, b=b_q: e.scalar_tensor_tensor(out=qs[:].rearrange("p h t -> p (h t)"), in0=psf[b][:, :],
                                                                   scalar=128.0 ** -0.5, in1=Eq, op0=ALU.mult, op1=ALU.mult),
                     reads=["ps%d" % b_q, "A0", "A1"], writes=["qs"])
                P.op("dve", lambda e, b=b_k: e.tensor_tensor(out=ks[:].rearrange("p h t -> p (h t)"), in0=psf[b][:, :], in1=Ek, op=ALU.mult),
                     reads=["ps%d" % b_k, "A2", "A3"], writes=["ks"])
                prel(b_q, b_k)
                yield "C4"
                b_kt = proj_tm(512)
                P.op("dve", lambda e, b=b_kt: e.tensor_tensor(out=kdec[:], in0=psf[b][:, :], in1=scrB[:], op=ALU.mult),
                     reads=["ps%d" % b_kt, "scrB"], writes=["kdec"])
                for half in range(2):
                    b = proj_tm(1024 + half * 512)
                    if half == 0:
                        P.op("act", lambda e, b=b, half=half: e.activation(out=vg[:, half * 512:(half + 1) * 512], in_=psf[b][:, :], func=AF.Copy),
                             reads=["ps%d" % b], writes=["vg%d" % half])
                    else:
                        P.op("dve", lambda e, b=b, half=half: e.tensor_copy(out=vg[:, half * 512:(half + 1) * 512], in_=psf[b][:, :]),
                             reads=["ps%d" % b], writes=["vg%d" % half])
                yield "TM"
                def headwise_fm(Wt, wkey, src, skeys, dst, dkeys, eng="act"):
                    for half in range(2):
                        b = palloc()
                        for cc in range(4):
                            c = half * 4 + cc
                            P.op("pe", lambda e, c=c, cc=cc, b=b: e.matmul(psf[b][:, cc * 128:(cc + 1) * 128], lhsT=Wt[:, c, :],
                                                                           rhs=src(c), start=True, stop=True),
                                 reads=[wkey] + skeys, writes=["ps%d" % b])
                        if eng == "act":
                            P.op("act", lambda e, half=half, b=b: e.activation(
                                out=dst[:, half * 4:(half + 1) * 4, :], in_=psf[b][:, :].rearrange("p (c t) -> p c t", c=4), func=AF.Copy),
                                reads=["ps%d" % b], writes=dkeys(half))
                        else:
                            P.op("dve", lambda e, half=half, b=b: e.tensor_copy(
                                out=dst[:, half * 4:(half + 1) * 4, :], in_=psf[b][:, :].rearrange("p (c t) -> p c t", c=4)),
                                reads=["ps%d" % b], writes=dkeys(half))

                CK = ["cactT%d" % c for c in range(8)]
                headwise_fm(Wq, "Wq", lambda c: cactT[:, c, :], CK, qmT, lambda half: ["qmT%d" % half])
                headwise_fm(Wk, "Wk", lambda c: cactT[:, c, :], CK, kmT, lambda half: ["kmT%d" % half], eng="dve")
                RK = ["R0", "R1", "R2", "R3"]
                headwise_fm(Wv, "Wv", lambda c: xme[:, c, 3:131], ["xme0", "xme1"], vmT, lambda half: ["oT0a"] if half == 0 else ["oT0b"])
                for half in range(2):
                    b = palloc()
                    for cc in range(4):
                        c = half * 4 + cc
                        P.op("pe", lambda e, c=c, cc=cc, b=b: e.matmul(psf[b][:, cc * 128:(cc + 1) * 128], lhsT=xme[:, c, 3:131],
                                                                       rhs=Wv[:, c, :], start=True, stop=True),
                             reads=["xme%d" % (c // 4), "Wv"], writes=["ps%d" % b])
                    if half == 0:
                        P.op("act", lambda e, half=half, b=b: e.activation(
                            out=vaug[:, half * 2:(half + 1) * 2, 0:256], in_=psf[b][:, :].rearrange("p (h e) -> p h e", h=2), func=AF.Copy),
                            reads=["ps%d" % b], writes=["vaug%d" % half])
                    else:
                        P.op("dve", lambda e, half=half, b=b: e.tensor_copy(
                            out=vaug[:, half * 2:(half + 1) * 2, 0:256], in_=psf[b][:, :].rearrange("p (h e) -> p h e", h=2)),
                            reads=["ps%d" % b], writes=["vaug%d" % half])
                if first:
                    P.op("pool", lambda e: e.memset(Sg32[:], 0.0), writes=["Sg32_%d" % h for h in range(4)])
                    P.op("pool", lambda e: e.memset(Sgb[:], 0.0), writes=["Sgb"])
                    P.op("pool", lambda e: e.memset(Cm32[:], 0.0), writes=["Cm32_%d" % c for c in range(8)])
                    P.op("pool", lambda e: e.memset(Cmb[:], 0.0), writes=["Cmb"])
                b_S = palloc()
                for h in range(4):
                    P.op("pe", lambda e, h=h, b=b_S: e.matmul(psf[b][:, h * 128:(h + 1) * 128], lhsT=ks[:, h, :], rhs=qs[:, h, :],
                                                              start=True, stop=True),
                         reads=["ks", "qs"], writes=["ps%d" % b_S])
                P.op("dve", lambda e, b=b_S: e.tensor_tensor(out=sm[:], in0=psf[b][:, :].rearrange("p (h t) -> p h t", h=4),
                                                             in1=maskb[:].unsqueeze(1).to_broadcast([128, 4, 128]), op=ALU.mult),
                     reads=["ps%d" % b_S, "maskb"], writes=["vmT0"])
                b_gt = 9
                if g == 0:
                    b_gb = palloc()
                    P.op("pe", lambda e, b=b_gb: e.matmul(psf[b][:, 0:8], lhsT=ones_row[0:1, :], rhs=gbias_row, start=True, stop=True),
                         reads=["ones_row", "rowsb"], writes=["ps%d" % b_gb])
                    P.op("dve", lambda e, b=b_gb: e.tensor_copy(out=gbb[:], in_=psf[b][:, 0:8]), reads=["ps%d" % b_gb], writes=["gbb"])
                srcs = [(qmT, ["qmT0", "qmT1"]), (kmT, ["kmT0", "kmT1"]), (vmT, ["oT0a", "oT0b"])]
                for c in range(24):
                    tsrc, tk = srcs[c // 8]
                    P.op("pe", lambda e, c=c, tsrc=tsrc, b=b_gt: e.matmul(psf[b][:, 0:8], lhsT=tsrc[:, c % 8, :], rhs=wg[:, c, :],
                                                                          start=(c == 0), stop=False),
                         reads=tk + ["wg"], writes=[PK(b_gt)])
                P.op("pe", lambda e, b=b_gt: e.matmul(psf[b][:, 0:8], lhsT=ident[:], rhs=gbb[:], start=False, stop=True),
                     reads=["ident", "gbb"], writes=[PK(b_gt)])
                P.op("act", lambda e, b=b_gt: e.activation(out=gtmp[:], in_=psf[b][:, 4:8], func=AF.Exp, scale=-1.0),
                     reads=[PK(b_gt)], writes=["gtmp"])
                P.op("act", lambda e: e.activation(out=gl[:, 0:4], in_=gtmp[:], func=AF.Ln, bias=1.0), reads=["gtmp"], writes=["gl0"])
                P.op("act", lambda e, b=b_gt: e.activation(out=gl[:, 4:8], in_=psf[b][:, 0:4], func=AF.Copy),
                     reads=[PK(b_gt)], writes=["gl1"])
                b_o = []
                for half in range(2):
                    b = palloc()
                    b_o.append(b)
                    for hh in range(2):
                        h = half * 2 + hh
                        P.op("pe", lambda e, h=h, hh=hh, b=b: e.matmul(psf[b][:, hh * 256:(hh + 1) * 256], lhsT=sm[:, h, :],
                                                                       rhs=vg[:, h * 256:(h + 1) * 256], start=True, stop=False),
                             reads=["vmT0", "vg0", "vg1"], writes=["ps%d" % b])
                        P.op("pe", lambda e, h=h, hh=hh, b=b: e.matmul(psf[b][:, hh * 256:(hh + 1) * 256], lhsT=qs[:, h, :],
                                                                       rhs=Sgb[:, h, :], start=False, stop=True),
                             reads=["qs", "Sgb"], writes=["ps%d" % b])
                for h in range(4):
                    b = b_o[h // 2]
                    hh = h % 2
                    P.op("act", lambda e, h=h, hh=hh, b=b: e.activation(out=junkh[h], in_=psf[b][:, hh * 256:(hh + 1) * 256],
                                                                        func=AF.Square, accum_out=st_g[:, h:h + 1]),
                         reads=["ps%d" % b], writes=["st_g0_%d" % h, "oT1_%d" % h])
                SG0 = ["st_g0_%d" % h for h in range(4)]
                P.op("act", lambda e: e.activation(out=st_g[:, 4:8], in_=st_g[:, 0:4], func=AF.Ln, scale=1.0 / 256, bias=EPS),
                     reads=SG0, writes=["st_g1"])
                P.op("act", lambda e: e.activation(out=st_g[:, 8:12], in_=st_g[:, 4:8], func=AF.Exp, scale=-0.5),
                     reads=["st_g1"], writes=["st_g2"])
                for h in range(4):
                    b = b_o[h // 2]
                    hh = h % 2
                    P.op("dve", lambda e, h=h, hh=hh, b=b: e.scalar_tensor_tensor(
                        out=obf[:, h * 256:(h + 1) * 256], in0=psf[b][:, hh * 256:(hh + 1) * 256], scalar=st_g[:, 8 + h:9 + h],
                        in1=szg[:, h * 256:(h + 1) * 256], op0=ALU.mult, op1=ALU.mult),
                        reads=["ps%d" % b, "st_g2", "szg0", "szg1"], writes=["obf_g%d" % h])
                for half in range(2):
                    b = palloc()
                    for hh in range(2):
                        h = half * 2 + hh
                        P.op("pe", lambda e, h=h, hh=hh, b=b: e.matmul(psf[b][:, hh * 256:(hh + 1) * 256], lhsT=kdec[:, h * 128:(h + 1) * 128],
                                                                       rhs=vg[:, h * 256:(h + 1) * 256], start=True, stop=True),
                             reads=["kdec", "vg0", "vg1"], writes=["ps%d" % b])
                    for hh in range(2):
                        h = half * 2 + hh
                        P.op("dve", lambda e, h=h, hh=hh, b=b: e.scalar_tensor_tensor(
                            out=Sg32[:, h, :], in0=Sg32[:, h, :], scalar=scrA[:, h * 128 + 127:h * 128 + 128],
                            in1=psf[b][:, hh * 256:(hh + 1) * 256], op0=ALU.mult, op1=ALU.add),
                            reads=["Sg32_%d" % h, "A0", "A1", "ps%d" % b], writes=["Sg32_%d" % h])

                b_kt2 = []
                for half in range(2):
                    b = palloc(avoid=b_kt2, hold=True)
                    b_kt2.append(b)
                    for cc in range(4):
                        c = half * 4 + cc
                        P.op("pe", lambda e, c=c, cc=cc, b=b: e.matmul(psf[b][:, cc * 128:(cc + 1) * 128], lhsT=cactT[:, c, :],
                                                                       rhs=Wk[:, c, :], start=True, stop=True),
                             reads=CK + ["Wk"], writes=["ps%d" % b])
                b_Sm = palloc(hold=True)
                for h in range(4):
                    for dc in range(2):
                        P.op("pe", lambda e, h=h, dc=dc, b=b_Sm: e.matmul(psf[b][:, h * 128:(h + 1) * 128], lhsT=kmT[:, 2 * h + dc, :],
                                                                          rhs=qmT[:, 2 * h + dc, :], start=(dc == 0), stop=(dc == 1)),
                             reads=["kmT0", "kmT1", "qmT0", "qmT1"], writes=["ps%d" % b_Sm])
                b_ga = 10
                ga = psf[b_ga]
                GL = ["gl0", "gl1"]
                P.op("pe", lambda e, ga=ga: e.matmul(ga[:, 0:4], lhsT=triN[:], rhs=gl[:, 0:4], start=True, stop=True),
                     reads=GL + ["triN"], writes=[PK(b_ga)])
                P.op("pe", lambda e, ga=ga: e.matmul(ga[:, 4:8], lhsT=onesN[:], rhs=gl[:, 0:4], start=True, stop=True),
                     reads=GL + ["onesN"], writes=[PK(b_ga)])
                P.op("pe", lambda e, ga=ga: e.matmul(ga[:, 8:12], lhsT=maskf[:], rhs=gl[:, 0:4], start=True, stop=False),
                     reads=GL + ["maskf"], writes=[PK(b_ga)])
                P.op("pe", lambda e, ga=ga: e.matmul(ga[:, 8:12], lhsT=identf[:], rhs=gl[:, 4:8], start=False, stop=True),
                     reads=GL + ["identf"], writes=[PK(b_ga)])
                P.op("pe", lambda e, ga=ga: e.matmul(ga[:, 12:16], lhsT=UN[:], rhs=gl[:, 0:4], start=True, stop=False),
                     reads=GL + ["UN"], writes=[PK(b_ga)])
                P.op("pe", lambda e, ga=ga: e.matmul(ga[:, 12:16], lhsT=identf[:], rhs=gl[:, 4:8], start=False, stop=True),
                     reads=GL + ["identf"], writes=[PK(b_ga)])
                P.op("act", lambda e, ga=ga: e.activation(out=gex[:, 0:8], in_=ga[:, 0:8], func=AF.Exp), reads=[PK(b_ga)], writes=["gex0"])
                P.op("act", lambda e, ga=ga: e.activation(out=gex[:, 8:16], in_=ga[:, 8:16], func=AF.Exp, bias=-LN16),
                     reads=[PK(b_ga)], writes=["gex1"])
                P.op("act", lambda e, ga=ga: e.activation(out=gex[:, 16:20], in_=ga[:, 0:4], func=AF.Exp, scale=-2.0),
                     reads=[PK(b_ga)], writes=["gex2"])
                yield "MS"
                for half in range(2):
                    b = b_kt2[half]
                    for hh in range(2):
                        h = half * 2 + hh
                        P.op("dve", lambda e, h=h, hh=hh, b=b: e.tensor_scalar(
                            out=kw[:, h * 256:(h + 1) * 256], in0=psf[b][:, hh * 256:(hh + 1) * 256],
                            scalar1=gex[:, 12 + h:13 + h], scalar2=None, op0=ALU.mult),
                            reads=["ps%d" % b, "gex1"], writes=["kw%d" % h])
                prel(*b_kt2)
                for h in range(4):
                    P.op("dve", lambda e, h=h, b=b_Sm: e.scalar_tensor_tensor(
                        out=smm[:, h, :], in0=psf[b][:, h * 128:(h + 1) * 128], scalar=gex[:, 8 + h:9 + h], in1=maskb[:],
                        op0=ALU.mult, op1=ALU.mult),
                        reads=["ps%d" % b_Sm, "gex1", "maskb"], writes=["R%d" % h])
                prel(b_Sm)
                b_P2 = [palloc(hold=True)]
                b_P2.append(palloc(avoid=b_P2, hold=True))
                b_den = 11
                PH = lambda h: psf[b_P2[h // 2]][:, (h % 2) * 256:(h % 2 + 1) * 256]
                for h in range(4):
                    pk = "ps%d" % b_P2[h // 2]
                    P.op("pe", lambda e, h=h: e.matmul(PH(h), lhsT=smm[:, h, :], rhs=vaug[:, h, 0:256], start=True, stop=False),
                         reads=["R%d" % h, "vaug0", "vaug1"], writes=[pk])
                    for dc in range(2):
                        P.op("pe", lambda e, h=h, dc=dc: e.matmul(PH(h), lhsT=qmT[:, 2 * h + dc, :], rhs=Cmb[:, 2 * h + dc, 0:256],
                                                                  start=False, stop=(dc == 1)),
                             reads=["qmT0", "qmT1", "Cmb"], writes=[pk])
                for h in range(4):
                    dk = PK(b_den)
                    P.op("pe", lambda e, h=h: e.matmul(psf[b_den][:, 2 * h:2 * h + 2], lhsT=smm[:, h, :], rhs=vaug[:, h, 255:257], start=True, stop=False),
                         reads=["R%d" % h, "vaug0", "vaug1"], writes=[dk])
                    for dc in range(2):
                        P.op("pe", lambda e, h=h, dc=dc: e.matmul(psf[b_den][:, 2 * h:2 * h + 2], lhsT=qmT[:, 2 * h + dc, :], rhs=Cmb[:, 2 * h + dc, 255:257],
                                                                  start=False, stop=(dc == 1)),
                             reads=["qmT0", "qmT1", "Cmb"], writes=[dk])
                b_P = b_P2 + [b_den]
                for h in range(4):
                    for dc in range(2):
                        b = palloc(avoid=b_P)
                        P.op("pe", lambda e, h=h, dc=dc, b=b: e.matmul(psf[b][:, 0:257], lhsT=kw[:, h * 256 + dc * 128:h * 256 + (dc + 1) * 128],
                                                                       rhs=vaug[:, h, :], start=True, stop=True),
                             reads=["kw%d" % h, "vaug0", "vaug1"], writes=["ps%d" % b])
                        P.op("dve", lambda e, h=h, dc=dc, b=b: e.scalar_tensor_tensor(
                            out=Cm32[:, 2 * h + dc, :], in0=Cm32[:, 2 * h + dc, :], scalar=gex[:, 4 + h:5 + h],
                            in1=psf[b][:, 0:257], op0=ALU.mult, op1=ALU.add),
                            reads=["Cm32_%d" % (2 * h + dc), "gex0", "ps%d" % b], writes=["Cm32_%d" % (2 * h + dc)])

                yield "MP"
                for h in range(4):
                    P.op("act", lambda e, h=h: e.activation(out=junkh[h], in_=PH(h), func=AF.Square, accum_out=st_m[:, h:h + 1]),
                         reads=["ps%d" % b_P2[h // 2]], writes=["st_m_ssq%d" % h, "oT1_%d" % h])
                P.op("act", lambda e: e.activation(out=st_m[:, 4:8], in_=psf[b_den][:, 0:8].rearrange("p (h two) -> p h two", two=2)[:, :, 1], func=AF.Square),
                     reads=[PK(b_den)], writes=["st_m_den"])
                SSQ = ["st_m_ssq%d" % h for h in range(4)]
                P.op("dve", lambda e: e.tensor_tensor(out=st_m[:, 8:12], in0=st_m[:, 4:8], in1=gex[:, 16:20], op=ALU.max),
                     reads=["st_m_den", "gex2"], writes=["st_m_D"])
                P.op("dve", lambda e: e.scalar_tensor_tensor(out=st_m[:, 24:28], in0=st_m[:, 8:12], scalar=EPS * 256.0, in1=st_m[:, 0:4],
                                                             op0=ALU.mult, op1=ALU.add),
                     reads=["st_m_D"] + SSQ, writes=["st_m_v"])
                P.op("act", lambda e: e.activation(out=st_m[:, 28:32], in_=st_m[:, 24:28], func=AF.Ln, scale=1.0 / 256),
                     reads=["st_m_v"], writes=["st_m_ln"])
                P.op("act", lambda e: e.activation(out=st_m[:, 36:40], in_=st_m[:, 28:32], func=AF.Exp, scale=-0.5),
                     reads=["st_m_ln"], writes=["st_m_s"])
                t1 = scrA
                for h in range(4):
                    P.op("dve", lambda e, h=h: e.scalar_tensor_tensor(
                        out=t1[:, h * 256:(h + 1) * 256], in0=PH(h), scalar=st_m[:, 36 + h:37 + h],
                        in1=gmn[:, h * 256:(h + 1) * 256], op0=ALU.mult, op1=ALU.mult),
                        reads=["ps%d" % b_P2[h // 2], "st_m_s", "gmn"], writes=["A%d" % h])
                prel(*b_P2)
                for half in range(2):
                    b = palloc()
                    for cc in range(4):
                        c = half * 4 + cc
                        P.op("pe", lambda e, c=c, cc=cc, b=b: e.matmul(psf[b][:, cc * 128:(cc + 1) * 128], lhsT=cactT[:, c, :],
                                                                       rhs=dskip[:, c, :], start=True, stop=True),
                             reads=CK + ["dskip"], writes=["ps%d" % b])
                    P.op("dve", lambda e, half=half, b=b: e.tensor_tensor(out=t1[:, half * 512:(half + 1) * 512], in0=psf[b][:, :],
                                                                          in1=t1[:, half * 512:(half + 1) * 512], op=ALU.add),
                         reads=["ps%d" % b, "A%d" % (2 * half), "A%d" % (2 * half + 1)], writes=["A%d" % (2 * half), "A%d" % (2 * half + 1)])
                    P.op("dve", lambda e, half=half: e.tensor_tensor(out=obf[:, D + half * 512:D + (half + 1) * 512],
                                                                     in0=t1[:, half * 512:(half + 1) * 512],
                                                                     in1=szm[:, half * 512:(half + 1) * 512], op=ALU.mult),
                         reads=["A%d" % (2 * half), "A%d" % (2 * half + 1), "szm%d" % half], writes=["obf_m%d" % half])
                P.op("act", lambda e: e.activation(out=Sgb[:].rearrange("p h e -> p (h e)"), in_=Sg32[:].rearrange("p h e -> p (h e)"), func=AF.Copy),
                     reads=["Sg32_%d" % h for h in range(4)], writes=["Sgb"])
                P.op("act", lambda e: e.activation(out=dmy[:, 1:2], in_=dmy[:, 0:1], func=AF.Silu), reads=["dmy0"], writes=["dmy1"])
                yield "PP"
                if g == 0:
                    for c in range(8):
                        P.op("dve", lambda e, c=c: e.tensor_scalar(out=wout[:, c, :], in0=wout[:, c, :], scalar1=p128[:, 8 + c:9 + c],
                                                                    scalar2=None, op0=ALU.mult),
                             reads=["wout0", "p128"], writes=["wout0"])
                OB = ["obf_g%d" % h for h in range(4)] + ["obf_m0", "obf_m1"]
                for half in range(2):
                    b = palloc()
                    for cc in range(8):
                        c = half * 8 + cc
                        P.op("pe", lambda e, c=c, cc=cc, b=b: e.transpose(out=psb[b][:, cc * 128:(cc + 1) * 128],
                                                                          in_=obf[:, c * 128:(c + 1) * 128], identity=ident[:]),
                             reads=(OB[0:4] if half == 0 else OB[4:6]) + ["ident"], writes=["ps%d" % b])
                    if half == 0:
                        P.op("act", lambda e, b=b: e.activation(out=oT[:, 0:8, :], in_=psb[b][:, 0:1024].rearrange("p (c t) -> p c t", c=8),
                                                                func=AF.Copy),
                             reads=["ps%d" % b], writes=["oT0a", "oT0b"])
                    else:
                        P.op("dve", lambda e, b=b: e.tensor_copy(out=oT[:, 8:16, :], in_=psb[b][:, 0:1024].rearrange("p (c t) -> p c t", c=8)),
                             reads=["ps%d" % b], writes=OTK[1:])
                P.op("dve", lambda e: e.tensor_copy(out=Cmb[:].rearrange("p h e -> p (h e)"), in_=Cm32[:].rearrange("p h e -> p (h e)")),
                     reads=["Cm32_%d" % c for c in range(8)], writes=["Cmb"])
                P.op("act", lambda e: e.activation(out=dmy[:, 3:4], in_=dmy[:, 2:3], func=AF.Exp), reads=["dmy2"], writes=["dmy3"])
                yield "FT"
                P.op("sp", lambda e, g=g: e.dma_start(out=yt[:], in_=x_d[g * 128:(g + 1) * 128, :]), writes=OB, chan="xr")
                b_y = [palloc()]
                b_y.append(palloc(avoid=b_y))
                for kk in range(2):
                    for half in range(2):
                        b = b_y[half]
                        for c in range(kk * 8, kk * 8 + 8):
                            P.op("pe", lambda e, c=c, half=half, b=b: e.matmul(psf[b][:, :], lhsT=oT[:, c, :],
                                                                               rhs=wout[:, c, half * 512:(half + 1) * 512],
                                                                               start=(c == 0), stop=(c == 15)),
                                 reads=((["oT0a"] if c < 4 else ["oT0b"]) + ["wout0"] if c < 8 else OTK[1:] + ["wout1"]), writes=["ps%d" % b])
                for half in range(2):
                    b = b_y[half]
                    P.op("dve", lambda e, half=half, b=b: e.tensor_tensor(out=yt[:, half * 512:(half + 1) * 512], in0=psf[b][:, :],
                                                                          in1=yt[:, half * 512:(half + 1) * 512], op=ALU.add),
                         reads=["ps%d" % b] + (OB[0:4] if half == 0 else OB[4:6]), writes=(OB[0:4] if half == 0 else OB[4:6]))
                yield "F"
                P.op("act", lambda e: e.activation(out=junk, in_=yt[:], func=AF.Square, accum_out=st_y[:, 0:1]),
                     reads=OB, writes=["st_y0", "oT0a", "oT0b"])
                P.op("act", lambda e: e.activation(out=st_y[:, 1:2], in_=st_y[:, 0:1], func=AF.Ln, scale=1.0 / D, bias=EPS),
                     reads=["st_y0"], writes=["st_y1"])
                P.op("act", lambda e: e.activation(out=st_y[:, 2:3], in_=st_y[:, 1:2], func=AF.Exp, scale=-0.5),
                     reads=["st_y1"], writes=["st_y2"])
                P.op("dve", lambda e: e.scalar_tensor_tensor(out=yt[:], in0=yt[:], scalar=st_y[:, 2:3], in1=gfin[:], op0=ALU.mult, op1=ALU.mult),
                     reads=OB + ["st_y2", "gfin"], writes=OB)
                ok = "out%d" % g
                out_keys.append(ok)
                P.op("sp", lambda e, g=g: e.dma_start(out=out_d[g * 128:(g + 1) * 128, :], in_=yt[:]),
                     reads=OB, writes=[ok], chan="st")

        tiles = [(s * ntile + ti, s, ti) for s in range(nseq) for ti in range(ntile)]
        gens = [tile(*t) for t in tiles]

        def run(gi, upto):
            if gi >= len(gens):
                return
            for tag in gens[gi]:
                if tag == upto:
                    return
            assert upto is None, (gi, upto)

        run(0, "A1"); run(0, "A"); run(0, "B1"); run(0, "CV"); run(0, "Z"); run(0, "QP"); run(1, "A1")
        for n in range(len(gens)):
            run(n, "TM")
            run(n + 1, "A")
            run(n, "MS")
            run(n + 1, "B1")
            run(n, "MP")
            run(n, "PP")
            run(n + 1, "CV")
            run(n + 1, "Z")
            run(n, "FT")
            run(n + 1, "QP")
            run(n + 2, "A1")
            run(n, None)

        P.op("sp", lambda e: e.nop(), reads=out_keys)
        P.emit()
    return nc


def prep_shared(norm_g, w_in, w_gla_gate_up, b_gla_gate, gla_norm_g, conv_w, conv_b,
                w_q_m, w_k_m, w_v_m, w_igate, b_igate, w_fgate, b_fgate,
                mlstm_norm_g, mlstm_skip, w_out, final_norm_g):
    f = lambda a: np.ascontiguousarray(np.asarray(a, dtype=np.float32))
    pp = lambda v: f(v).reshape(8, 128).T
    p128 = np.concatenate([pp(norm_g), pp(f(gla_norm_g).reshape(-1)), pp(mlstm_skip)]
                          + [pp(f(conv_w)[k]) for k in range(4)] + [pp(conv_b)], axis=1)
    rows = np.concatenate([f(conv_b).reshape(1, -1), f(b_igate).reshape(1, -1), f(b_fgate).reshape(1, -1)], axis=1)
    wup = np.concatenate([f(w_gla_gate_up), f(b_gla_gate).reshape(1, -1), np.zeros((111, 512), np.float32)], axis=0)

    def expand(w):
        w = f(w)
        o = np.zeros((1024, 128), np.float32)
        for n in range(256):
            c, nl = divmod(n, 32)
            o[c * 128 + 4 * nl:c * 128 + 4 * nl + 4, 4 * nl:4 * nl + 4] = w[n]
        return o

    wg = np.concatenate([f(w_igate), f(w_fgate)], axis=1)
    wg = wg.reshape(24, 128, 8).transpose(1, 0, 2).reshape(128, 192)
    return {
        "w_in": f(w_in), "w_out": f(w_out), "p128": f(p128), "rows": f(rows), "wup": f(wup),
        "wq": expand(w_q_m), "wk": expand(w_k_m), "wv": expand(w_v_m), "wg": f(wg),
        "gmn": f(mlstm_norm_g).reshape(1, -1), "gfin": f(final_norm_g).reshape(1, -1),
    }


_NC_CACHE = {}


def kernel(x, **params):
    x = np.asarray(x, dtype=np.float32)
    bsz, seq, _ = x.shape
    nseq = bsz // NCORES
    ntile = seq // 128
    key = (nseq, ntile)
    if key not in _NC_CACHE:
        _NC_CACHE[key] = build(nseq, ntile)
    nc = _NC_CACHE[key]
    shared = prep_shared(**params)
    in_maps = []
    for c in range(NCORES):
        m = dict(shared)
        m["x"] = np.ascontiguousarray(x[c * nseq:(c + 1) * nseq].reshape(nseq * seq, D))
        in_maps.append(m)
    res = run_bass_kernel_spmd(nc, in_maps, core_ids=list(range(NCORES)))
    outs = [np.asarray(r["out"]).reshape(nseq, seq, D) for r in res.results]
    return np.concatenate(outs, axis=0).astype(np.float32)
```
